# Optimizing a Trainium2 kernel written in Bass

```python
import jax, jax.numpy as jnp
from jax import lax
import numpy as np

D_MODEL = 4096
BATCH = 2
SEQ = 8192
DEPTH = 2

HEAD_DIM = 128
MOBA_HEADS = 16
SB_HEADS = 16
MOBA_WIDTH = MOBA_HEADS * HEAD_DIM
SB_WIDTH = SB_HEADS * HEAD_DIM
MOBA_BLOCK = 256
MOBA_TOPK = 3
MOBA_Q_CHUNK = 32
SB_Q_BLOCK = 128
D_FF = 4 * D_MODEL
N_BRANCHES = 2
IN_COLS = 3 * MOBA_WIDTH + 3 * SB_WIDTH + N_BRANCHES * D_MODEL
RMS_EPS = 1e-6
NEG = -1e30

kernel_name = "moba_stickbreaking_gated_hybrid"


def rms_norm(x, g):
    xf = x.astype(jnp.float32)
    y = xf * lax.rsqrt(jnp.mean(xf * xf, axis=-1, keepdims=True) + RMS_EPS)
    return (y * g.astype(jnp.float32)).astype(x.dtype)


def alibi_slopes(n_heads):
    return jnp.exp2(-8.0 * jnp.arange(1, n_heads + 1, dtype=jnp.float32) / n_heads)


def to_heads(t, n_heads):
    b, s, _ = t.shape
    return t.reshape(b, s, n_heads, HEAD_DIM).transpose(0, 2, 1, 3)


def from_heads(t):
    b, h, s, d = t.shape
    return t.transpose(0, 2, 1, 3).reshape(b, s, h * d)


def moba_attention(q, k, v, slopes):
    B, H, S, Dh = q.shape
    nb = -(-S // MOBA_BLOCK)
    s_pad = nb * MOBA_BLOCK
    pad = ((0, 0), (0, 0), (0, s_pad - S), (0, 0))
    kp = jnp.pad(k, pad)
    vp = jnp.pad(v, pad)
    k_blocks = kp.reshape(B, H, nb, MOBA_BLOCK, Dh)
    v_blocks = vp.reshape(B, H, nb, MOBA_BLOCK, Dh)
    k_mean = jnp.mean(k_blocks.astype(jnp.float32), axis=3)
    topk = min(MOBA_TOPK, nb)
    scale = Dh ** -0.5
    n_chunks = S // MOBA_Q_CHUNK
    q_chunks = q.reshape(B, H, n_chunks, MOBA_Q_CHUNK, Dh).transpose(2, 0, 1, 3, 4)
    b_idx = jnp.arange(B)[:, None, None, None]
    h_idx = jnp.arange(H)[None, :, None, None]
    blk_pos = jnp.arange(MOBA_BLOCK)
    block_ids = jnp.arange(nb)
    sl5 = slopes[:, None, None, None]
    sl4 = slopes[:, None, None]

    def one_chunk(args):
        c, qc = args
        t = c * MOBA_Q_CHUNK + jnp.arange(MOBA_Q_CHUNK)
        own = (c * MOBA_Q_CHUNK) // MOBA_BLOCK
        gate = jnp.einsum('bhqd,bhnd->bhqn', qc.astype(jnp.float32), k_mean)
        gate = jnp.where(block_ids < own, gate, NEG)
        _, sel = lax.top_k(gate, topk)
        sel_valid = sel < own
        k_sel = k_blocks[b_idx, h_idx, sel]
        v_sel = v_blocks[b_idx, h_idx, sel]
        s_sel = jnp.einsum('bhqd,bhqrkd->bhqrk', qc, k_sel).astype(jnp.float32) * scale
        pos_sel = sel[..., None] * MOBA_BLOCK + blk_pos
        s_sel = s_sel - sl5 * (t[:, None, None] - pos_sel)
        s_sel = jnp.where(sel_valid[..., None], s_sel, NEG)
        k_own = lax.dynamic_slice_in_dim(kp, own * MOBA_BLOCK, MOBA_BLOCK, axis=2)
        v_own = lax.dynamic_slice_in_dim(vp, own * MOBA_BLOCK, MOBA_BLOCK, axis=2)
        dist = t[:, None] - (own * MOBA_BLOCK + blk_pos)[None, :]
        s_own = jnp.einsum('bhqd,bhkd->bhqk', qc, k_own).astype(jnp.float32) * scale
        s_own = jnp.where(dist >= 0, s_own - sl4 * dist, NEG)
        scores = jnp.concatenate([s_sel.reshape(B, H, MOBA_Q_CHUNK, topk * MOBA_BLOCK), s_own], axis=-1)
        p = jax.nn.softmax(scores, axis=-1)
        p_sel = p[..., :topk * MOBA_BLOCK].reshape(B, H, MOBA_Q_CHUNK, topk, MOBA_BLOCK).astype(v.dtype)
        p_own = p[..., topk * MOBA_BLOCK:].astype(v.dtype)
        return (jnp.einsum('bhqrk,bhqrkd->bhqd', p_sel, v_sel)
                + jnp.einsum('bhqk,bhkd->bhqd', p_own, v_own))

    out = lax.map(one_chunk, (jnp.arange(n_chunks), q_chunks))
    return out.transpose(1, 2, 0, 3, 4).reshape(B, H, S, Dh)


def stick_breaking_attention(q, k, v):
    B, H, S, Dh = q.shape
    scale = Dh ** -0.5
    nq = S // SB_Q_BLOCK
    q_blocks = q.reshape(B, H, nq, SB_Q_BLOCK, Dh).transpose(2, 0, 1, 3, 4)
    key_pos = jnp.arange(S)

    def one_block(args):
        c, qb = args
        t = c * SB_Q_BLOCK + jnp.arange(SB_Q_BLOCK)
        z = jnp.einsum('bhqd,bhkd->bhqk', qb, k).astype(jnp.float32) * scale
        strict = key_pos[None, :] < t[:, None]
        log_beta = jax.nn.log_sigmoid(z)
        log_1m = jnp.where(strict, jax.nn.log_sigmoid(-z), 0.0)
        later = lax.cumsum(log_1m, axis=3, reverse=True) - log_1m
        w = jnp.where(strict, jnp.exp(log_beta + later), 0.0)
        return jnp.einsum('bhqk,bhkd->bhqd', w.astype(v.dtype), v)

    out = lax.map(one_block, (jnp.arange(nq), q_blocks))
    return out.transpose(1, 2, 0, 3, 4).reshape(B, H, S, Dh)


def hybrid_layer(x, g_mix, w_in, b_gate, g_q, g_k, w_br_moba, w_br_sb, w_out, g_mlp, w_up, w_down, slopes):
    h = rms_norm(x, g_mix)
    proj = h @ w_in
    o = np.cumsum([0, MOBA_WIDTH, MOBA_WIDTH, MOBA_WIDTH, SB_WIDTH, SB_WIDTH, SB_WIDTH, D_MODEL, D_MODEL])
    qa, ka, va, qb, kb, vb, ga, gb = [proj[..., o[i]:o[i + 1]] for i in range(8)]
    qa = rms_norm(to_heads(qa, MOBA_HEADS), g_q)
    ka = rms_norm(to_heads(ka, MOBA_HEADS), g_k)
    ya = from_heads(moba_attention(qa, ka, to_heads(va, MOBA_HEADS), slopes))
    yb = from_heads(stick_breaking_attention(to_heads(qb, SB_HEADS), to_heads(kb, SB_HEADS),
                                             to_heads(vb, SB_HEADS)))
    gates = jax.nn.sigmoid((jnp.concatenate([ga, gb], axis=-1) + b_gate).astype(jnp.float32)).astype(x.dtype)
    merged = gates[..., :D_MODEL] * (ya @ w_br_moba) + gates[..., D_MODEL:] * (yb @ w_br_sb)
    x = x + merged @ w_out
    h2 = rms_norm(x, g_mlp)
    return x + jnp.square(jax.nn.relu(h2 @ w_up)) @ w_down


def setup_inputs(seed: int = 0) -> dict:
    key = jax.random.key(seed)
    ks = jax.random.split(key, 12)
    f32 = jnp.float32
    nrm = lambda k, shape, s: jax.random.normal(k, shape, f32) * s
    return {
        "x": nrm(ks[0], (BATCH, SEQ, D_MODEL), 1.0),
        "norm_mix": 1.0 + nrm(ks[1], (DEPTH, D_MODEL), 0.02),
        "w_in": nrm(ks[2], (DEPTH, D_MODEL, IN_COLS), D_MODEL ** -0.5),
        "b_gate": nrm(ks[3], (DEPTH, N_BRANCHES * D_MODEL), 0.02),
        "q_norm": 1.0 + nrm(ks[4], (DEPTH, HEAD_DIM), 0.02),
        "k_norm": 1.0 + nrm(ks[5], (DEPTH, HEAD_DIM), 0.02),
        "w_branch_moba": nrm(ks[6], (DEPTH, MOBA_WIDTH, D_MODEL), MOBA_WIDTH ** -0.5),
        "w_branch_sb": nrm(ks[7], (DEPTH, SB_WIDTH, D_MODEL), SB_WIDTH ** -0.5),
        "w_out": nrm(ks[8], (DEPTH, D_MODEL, D_MODEL), D_MODEL ** -0.5),
        "norm_mlp": 1.0 + nrm(ks[9], (DEPTH, D_MODEL), 0.02),
        "w_up": nrm(ks[10], (DEPTH, D_MODEL, D_FF), D_MODEL ** -0.5),
        "w_down": nrm(ks[11], (DEPTH, D_FF, D_MODEL), D_FF ** -0.5),
    }


def reference(x, norm_mix, w_in, b_gate, q_norm, k_norm, w_branch_moba, w_branch_sb, w_out,
              norm_mlp, w_up, w_down):
    slopes = alibi_slopes(MOBA_HEADS)
    for l in range(DEPTH):
        x = hybrid_layer(x, norm_mix[l], w_in[l], b_gate[l], q_norm[l], k_norm[l], w_branch_moba[l],
                         w_branch_sb[l], w_out[l], norm_mlp[l], w_up[l], w_down[l], slopes)
    return x
```

```python
import numpy as np
from contextlib import ExitStack
import concourse.bass as bass
import concourse.mybir as mybir
from concourse.bass_utils import run_bass_kernel_spmd

F32 = mybir.dt.float32
BF16 = mybir.dt.bfloat16
AF = mybir.ActivationFunctionType
ALU = mybir.AluOpType
AX = mybir.AxisListType
ENG = ('pe', 'act', 'dve', 'pool', 'sp')
EPS = 1e-6
NEGM = -30000.0


class Cfg:
    def __init__(self, D=4096, NH=16, S=8192, DFF=16384, NL=2):
        self.D, self.NH, self.S, self.DFF, self.NL = D, NH, S, DFF, NL
        self.TC = S // 2
        self.KC = D // 128
        self.WD = NH * 128
        self.KCW = NH
        self.INC = 6 * self.WD + 2 * D
        self.NB = S // 256
        self.NT = self.TC // 512
        self.NHC = NH // 2
        self.NQT = S // 512
        self.NKT = S // 128
        self.FCH = min(DFF, 4096)
        self.NFC = DFF // self.FCH


class Prog:
    def __init__(self, nc, stack):
        self.nc, self.stack = nc, stack
        self.sems, self.count = {}, {}
        self.ops = {e: [] for e in ENG}
        self.waited = {e: {} for e in ENG}
        self.Wt, self.Rd = {}, {}
        self.nbar = 0
        for e in ('pe', 'act', 'dve', 'pool'):
            self._sem('e_' + e)

    def _sem(self, name):
        if name not in self.sems:
            self.sems[name] = self.stack.enter_context(self.nc.semaphore(name))
            self.count[name] = 0
        return self.sems[name]

    def op(self, eng, emit, reads=(), writes=(), dma=None):
        deps = {}

        def add(d):
            for s, v in d.items():
                if deps.get(s, 0) < v:
                    deps[s] = v
        for r in reads:
            add(self.Wt.get(r, {}))
        for w in writes:
            add(self.Wt.get(w, {}))
            add(self.Rd.get(w, {}))
        if dma is not None:
            sname = 'd_' + dma
            self._sem(sname)
            if self.count[sname] > 0:
                add({sname: self.count[sname]})
            inc = 16
        else:
            sname = 'e_' + eng
            inc = 1
        self.count[sname] += inc
        tok = self.count[sname]
        waits = []
        wd = self.waited[eng]
        for s, v in deps.items():
            if eng == 'pe' and s == 'e_pe':
                continue
            if wd.get(s, 0) >= v:
                continue
            wd[s] = v
            waits.append((s, v))
        self.ops[eng].append((waits, emit, sname, inc))
        for r in reads:
            d = self.Rd.setdefault(r, {})
            d[sname] = max(d.get(sname, 0), tok)
        for w in writes:
            d = self.Wt.setdefault(w, {})
            d[sname] = max(d.get(sname, 0), tok)

    def flush(self):
        nc = self.nc
        endw = [(s, v) for s, v in self.count.items() if v > 0]
        ops = self.ops
        sems = self.sems
        waited = self.waited

        def mk(en):
            def f(e):
                for waits, emit, sname, inc in ops[en]:
                    for s, v in waits:
                        e.wait_ge(sems[s], v)
                    ins = emit(e)
                    ins.then_inc(sems[sname], inc)
                for s, v in endw:
                    if waited[en].get(s, 0) < v:
                        e.wait_ge(sems[s], v)
                        waited[en][s] = v
            return f
        with nc.Block() as block:
            block.tensor(mk('pe'))
            block.scalar(mk('act'))
            block.vector(mk('dve'))
            block.gpsimd(mk('pool'))
            block.sync(mk('sp'))
        self.ops = {e: [] for e in ENG}

    def barrier(self):
        self.flush()
        nc = self.nc
        self.nbar += 1
        k = self.nbar
        flags, barv, rk = self.flags, self.barv, self.rk_sp
        sdma, sdone = self._sem('bar_dma'), self._sem('bar_done')
        with nc.Block() as block:
            def spf(e):
                e.dma_start(out=flags[rk, 0:1].rearrange("o c -> (o c)"), in_=barv[0, k:k + 1]).then_inc(sdma, 16)
                e.wait_ge(sdma, 16 * k)
                with e.register(f"bv{k}") as v, e.register(f"bf0{k}") as f0, e.register(f"bf1{k}") as f1, e.register(f"bc{k}") as c:
                    e.load(v, barv[0:1, k:k + 1])
                    e.reg_mov(c, 1)
                    with e.While(c):
                        e.load(f0, flags[0:1, 0:1])
                        e.load(f1, flags[1:2, 0:1])
                        e.reg_alu(f0, f0, v, ALU.bitwise_xor)
                        e.reg_alu(f1, f1, v, ALU.bitwise_xor)
                        e.reg_alu(c, f0, f1, ALU.bitwise_or)
                e.sem_inc(sdone, 1)

            def other(e):
                e.wait_ge(sdone, k)
            block.sync(spf)
            block.tensor(other)
            block.scalar(other)
            block.vector(other)
            block.gpsimd(other)


class Ring:
    def __init__(self, items):
        self.items, self.i = list(items), 0

    def next(self):
        it = self.items[self.i % len(self.items)]
        self.i += 1
        return it


def build(cfg, PARTS=('A', 'moba', 'sb', 'C'), dbg=False):
    D, NH, S, DFF, NL = cfg.D, cfg.NH, cfg.S, cfg.DFF, cfg.NL
    TC, KC, WD, KCW, INC, NB, NT, NHC = cfg.TC, cfg.KC, cfg.WD, cfg.KCW, cfg.INC, cfg.NB, cfg.NT, cfg.NHC
    NQT, NKT, FCH, NFC = cfg.NQT, cfg.NKT, cfg.FCH, cfg.NFC
    SCALE = 128.0 ** -0.5
    nc = bass.Bass("TRN2", target_bir_lowering=False, num_devices=8)
    stack = ExitStack()
    P = Prog(nc, stack)

    def din(name, shape, dt=F32):
        return nc.dram_tensor(name, list(shape), dt, kind="ExternalInput").ap()

    def dint(name, shape, dt, shared=False):
        return nc.dram_tensor(name, list(shape), dt, addr_space=("Shared" if shared else "Local")).ap()

    xT = din("xT", [D, TC])
    wnames = [("win", D, INC), ("wbm", WD, D), ("wbs", WD, D), ("wout", D, D), ("wup", D, DFF), ("wdn", DFF, D)]
    w_in32 = {}
    for l in range(NL):
        for nm, K, M in wnames:
            w_in32[(nm, l)] = din(f"{nm}{l}", [K // 2, M])
    gmix_d = din("gmix", [128, NL * KC])
    gmlp_d = din("gmlp", [128, NL * KC])
    bgate_d = din("bgate", [128, NL * 2 * KC])
    gqk_d = din("gqk", [128, NL * 2])
    bcol_d = din("bcol", [128, NHC * 64])
    rw_d = din("rw", [128, 3 * 64])
    esel_d = din("esel", [35, NB * 128], BF16)
    qrow_d = din("qrow", [35, NHC * 512], BF16)
    caus_d = din("caus", [128, 2 * 4 * 512], BF16)
    cst_d = din("cst", [128, 4 * 128], BF16)
    onesf_d = din("onesf", [128, 128])
    outT = nc.dram_tensor("outT", [D, TC], F32, kind="ExternalOutput").ap()

    wb = {}
    for l in range(NL):
        for nm, K, M in wnames:
            wb[(nm, l)] = dint(f"wb_{nm}{l}", [2, K // 2, M], BF16, shared=True)
    SQK = dint("SQK", [4, 2, 2, NHC, 128, TC], BF16, True)
    SV = dint("SV", [2, 2, TC, 2, NHC * 128], BF16, True)
    SY = dint("SY", [2, 2, NHC * 128, 2, TC], BF16, True)
    LQK = dint("LQK", [4, NH, 128, TC], BF16)
    LV = dint("LV", [2, TC, WD], BF16)
    MQK = dint("MQK", [4, 2, NHC, 128, TC], BF16)
    MV = dint("MV", [2, 2, TC, NHC * 128], BF16)
    LY = dint("LY", [2, NHC * 128, 2, TC], BF16)
    CY = dint("CY", [2, 2, NHC * 128, TC], BF16)
    G = dint("G", [2 * D, TC], BF16)
    X1 = dint("X1", [D, TC], F32)

    pid = nc.gpsimd.partition_id()
    rk_pool = bass.ds(pid % 2, 1)
    rk_sp = bass.ds(nc.sync.partition_id() % 2, 1)
    rk_act = bass.ds(nc.scalar.partition_id() % 2, 1)
    I32 = mybir.dt.int32
    P.flags = nc.dram_tensor("barflags", [2, 16], I32, addr_space="Shared").ap()
    P.barv = nc.dram_tensor("barv", [1, 16], I32, kind="ExternalInput").ap()
    P.rk_sp = rk_sp

    _uid = [0]

    def sb(name, shape, dt, st=stack):
        _uid[0] += 1
        return st.enter_context(nc.sbuf_tensor(f"{name}_{_uid[0]}", list(shape), dt))

    gmix = sb("gmix_s", [128, NL * KC], F32)
    gmlp = sb("gmlp_s", [128, NL * KC], F32)
    bgate = sb("bgate_s", [128, NL * 2 * KC], F32)
    gqk = sb("gqk_s", [128, NL * 2], F32)
    cst = sb("cst_s", [128, 4 * 128], BF16)
    onesf = sb("onesf_s", [128, 128], F32)
    IDENT, TRI, NTRI, ONES = (cst[:, i * 128:(i + 1) * 128] for i in range(4))

    ps = [stack.enter_context(nc.psum_tensor(f"ps{i}", [128, 512], F32)) for i in range(8)]

    for i, (dst, src) in enumerate([(gmix, gmix_d), (gmlp, gmlp_d), (bgate, bgate_d), (gqk, gqk_d), (cst, cst_d), (onesf, onesf_d)]):
        P.op('sp', (lambda e, d=dst, s=src: e.dma_start(out=d[:], in_=s)), writes=[f"const{i}"], dma=f"const{i}")
    CONSTS = [f"const{i}" for i in range(6)]

    wi = 0
    for l in range(NL):
        for nm, K, M in wnames:
            src = w_in32[(nm, l)]
            dst = wb[(nm, l)][rk_pool, :, :].rearrange("o k m -> (o k) m")
            key = f"wc{wi % 4}"
            wi += 1
            P.op('pool', (lambda e, d=dst, s=src: e.dma_start(out=d, in_=s)), writes=[f"wb_{nm}{l}"], dma=key)
    P.barrier()

    def rmsnorm(xt, ht, gtab, goff, sqr, rtr, psn, tagx, tagh):
        pn = psn.next()
        G4 = 4 if KC % 4 == 0 else (2 if KC % 2 == 0 else 1)
        ngr = KC // G4
        for g in range(ngr):
            sq, sqn = sqr.next()
            P.op('act', (lambda e, sq=sq, g=g: e.activation(out=sq[:, 0:G4, :], in_=xt[:, g * G4:(g + 1) * G4, :], func=AF.Square)),
                 reads=[tagx], writes=[sqn])

            def mm(e, sq=sq, g=g, pn=pn):
                ins = None
                for j in range(G4):
                    ins = e.matmul(pn[0][:, :], onesf[:, :], sq[:, j, :], start=(g == 0 and j == 0), stop=(g == ngr - 1 and j == G4 - 1))
                return ins
            P.op('pe', mm, reads=[sqn, CONSTS[5]], writes=[pn[1]])
        rt, rtn = rtr.next()
        P.op('act', (lambda e: e.activation(out=rt[:, :], in_=pn[0][:, :], func=AF.Sqrt, scale=1.0 / D, bias=epsb[:, 0:1])),
             reads=[pn[1], 'epsb'], writes=[rtn])
        P.op('dve', (lambda e: e.reciprocal(out=rt[:, :], in_=rt[:, :])), reads=[rtn], writes=[rtn])
        for kc in range(KC):
            eng = 'dve'
            P.op(eng, (lambda e, kc=kc: e.scalar_tensor_tensor(out=ht[:, kc, :], in0=xt[:, kc, :], scalar=gtab[:, goff + kc:goff + kc + 1],
                                                                 in1=rt[:, :], op0=ALU.mult, op1=ALU.mult)),
                 reads=[tagx, rtn] + CONSTS[:2], writes=[tagh])

    def gemm(act, tagact, KCa, wdram, tagw, row0, col0, ncols, GW, wring, psring, epi, tokmajor=None):
        for g0 in range(0, ncols, GW):
            gw = min(GW, ncols - g0)
            wt, wtn = wring.next()
            src = wdram.rearrange("r k m -> (r k) m")[row0:row0 + KCa * 128, col0 + g0:col0 + g0 + gw].rearrange("(kc p) m -> p kc m", p=128)
            P.op('sp', (lambda e, wt=wt, src=src, gw=gw: e.dma_start(out=wt[:, 0:KCa, 0:gw], in_=src)),
                 reads=[tagw], writes=[wtn], dma=wtn)
            for m0 in range(0, gw, 128):
                mt = (g0 + m0) // 128
                pb = psring.next()
                if tokmajor is not None and tokmajor(mt):
                    def mm(e, wt=wt, m0=m0, pb=pb):
                        ins = None
                        for ts in range(4):
                            for kc in range(KCa):
                                ins = e.matmul(pb[0][:, ts * 128:(ts + 1) * 128], act[:, kc, ts * 128:(ts + 1) * 128], wt[:, kc, m0:m0 + 128],
                                               start=(kc == 0), stop=(kc == KCa - 1))
                        return ins
                else:
                    def mm(e, wt=wt, m0=m0, pb=pb):
                        ins = None
                        for kc in range(KCa):
                            ins = e.matmul(pb[0][:, :], wt[:, kc, m0:m0 + 128], act[:, kc, :], start=(kc == 0), stop=(kc == KCa - 1))
                        return ins
                P.op('pe', mm, reads=[wtn, tagact], writes=[pb[1]])
                epi(mt, pb)

    epsb = sb("epsb", [128, 1], F32)
    P.op('dve', (lambda e: e.memset(epsb[:, :], EPS)), writes=['epsb'])

    def phase_A(l, xsrc):
        st = ExitStack()
        xt = sb("A_xt", [128, KC, 512], F32, st)
        ht = sb("A_ht", [128, KC, 512], BF16, st)
        wr = Ring([(sb(f"A_w{i}", [128, KC, 512], BF16, st), f"A_w{i}") for i in range(2)])
        sqr = Ring([(sb(f"A_sq{i}", [128, 4, 512], F32, st), f"A_sq{i}") for i in range(2)])
        rtr = Ring([(sb(f"A_rt{i}", [128, 512], F32, st), f"A_rt{i}") for i in range(2)])
        sq1 = Ring([(sb(f"A_q{i}", [128, 512], F32, st), f"A_q{i}") for i in range(2)])
        stg = Ring([(sb(f"A_st{i}", [128, 512], BF16, st), f"A_st{i}") for i in range(4)])
        psg = Ring([(ps[i], f"ps{i}") for i in range(4)])
        psn = Ring([(ps[i], f"ps{i}") for i in (4, 5)])
        psq = Ring([(ps[i], f"ps{i}") for i in (6, 7)])
        segs = [("qa", WD), ("ka", WD), ("va", WD), ("qb", WD), ("kb", WD), ("vb", WD), ("ga", D), ("gb", D)]
        bounds = np.cumsum([0] + [s[1] for s in segs])

        def seg_of(mt):
            c = mt * 128
            i = int(np.searchsorted(bounds, c, side='right') - 1)
            return segs[i][0], (c - bounds[i]) // 128

        for tp in range(NT):
            t0 = tp * 512
            src = xsrc[:, t0:t0 + 512].rearrange("(kc p) t -> p kc t", p=128)
            P.op('sp', (lambda e, src=src: e.dma_start(out=xt[:, :, :], in_=src)), reads=['X'], writes=['A_xt'], dma='A_xt')
            rmsnorm(xt, ht, gmix, l * KC, sqr, rtr, psn, 'A_xt', 'A_ht')

            def epi(mt, pb, t0=t0):
                kind, j = seg_of(mt)
                so, son = stg.next()
                if kind in ('qa', 'ka'):
                    q1, q1n = sq1.next()
                    P.op('act', (lambda e: e.activation(out=q1[:, :], in_=pb[0][:, :], func=AF.Square)), reads=[pb[1]], writes=[q1n])
                    pq = psq.next()
                    P.op('pe', (lambda e: e.matmul(pq[0][:, :], onesf[:, :], q1[:, :], start=True, stop=True)), reads=[q1n, CONSTS[5]], writes=[pq[1]])
                    P.op('act', (lambda e: e.activation(out=q1[:, :], in_=pq[0][:, :], func=AF.Sqrt, scale=1.0 / 128, bias=epsb[:, 0:1])),
                         reads=[pq[1], 'epsb'], writes=[q1n])
                    P.op('dve', (lambda e: e.reciprocal(out=q1[:, :], in_=q1[:, :])), reads=[q1n], writes=[q1n])
                    gi = l * 2 + (0 if kind == 'qa' else 1)
                    P.op('dve', (lambda e: e.scalar_tensor_tensor(out=so[:, :], in0=pb[0][:, :], scalar=gqk[:, gi:gi + 1], in1=q1[:, :],
                                                                  op0=ALU.mult, op1=ALU.mult)),
                         reads=[pb[1], q1n, CONSTS[3]], writes=[son])
                    dst = LQK[0 if kind == 'qa' else 1, j, :, t0:t0 + 512]
                    P.op('sp', (lambda e: e.dma_start(out=dst, in_=so[:, :])), reads=[son], writes=['QKV'], dma=son)
                elif kind in ('qb', 'kb'):
                    sc = SCALE if kind == 'qb' else 1.0
                    P.op('act', (lambda e: e.activation(out=so[:, :], in_=pb[0][:, :], func=AF.Copy, scale=sc)), reads=[pb[1]], writes=[son])
                    dst = LQK[2 if kind == 'qb' else 3, j, :, t0:t0 + 512]
                    P.op('sp', (lambda e: e.dma_start(out=dst, in_=so[:, :])), reads=[son], writes=['QKV'], dma=son)
                elif kind in ('va', 'vb'):
                    P.op('dve', (lambda e: e.tensor_copy(out=so[:, :], in_=pb[0][:, :])), reads=[pb[1]], writes=[son])
                    dst = LV[0 if kind == 'va' else 1, t0:t0 + 512, j * 128:(j + 1) * 128].rearrange("(ts p) d -> p ts d", p=128)
                    P.op('sp', (lambda e: e.dma_start(out=dst, in_=so[:, :].rearrange("p (ts d) -> p ts d", d=128))),
                         reads=[son], writes=['QKV'], dma=son)
                else:
                    gofs = l * 2 * KC + (0 if kind == 'ga' else KC) + j
                    P.op('act', (lambda e: e.activation(out=so[:, :], in_=pb[0][:, :], func=AF.Sigmoid, bias=bgate[:, gofs:gofs + 1])),
                         reads=[pb[1], CONSTS[2]], writes=[son])
                    row = (0 if kind == 'ga' else D) + j * 128
                    dst = G[row:row + 128, t0:t0 + 512]
                    P.op('sp', (lambda e: e.dma_start(out=dst, in_=so[:, :])), reads=[son], writes=['G'], dma=son)

            gemm(ht, 'A_ht', KC, wb[("win", l)], f"wb_win{l}", 0, 0, INC, 512, wr, psg, epi,
                 tokmajor=lambda mt: seg_of(mt)[0] in ('va', 'vb'))
        P.op('act', (lambda e: e.dma_start(out=SQK[:, rk_act, :, :, :, :].rearrange("q o hr hh d t -> q (o hr) hh d t"),
                                          in_=LQK.rearrange("q (hr hh) d t -> q hr hh d t", hr=2))), reads=['QKV'], writes=['SQKV'], dma='xq')
        P.op('act', (lambda e: e.dma_start(out=SV[:, rk_act, :, :, :].rearrange("v o t hr c -> v (o t) hr c"),
                                          in_=LV.rearrange("v t (hr c) -> v t hr c", hr=2))), reads=['QKV'], writes=['SQKV'], dma='xv')
        P.flush()
        st.close()

    def phase_B(l):
        st = ExitStack()
        kTr = Ring([(sb(f"B_k{i}", [128, S], BF16, st), f"B_k{i}") for i in range(2)])
        qTr = Ring([(sb(f"B_q{i}", [128, S], BF16, st), f"B_q{i}") for i in range(2)])
        vr = Ring([(sb(f"B_v{i}", [128, NKT, 128], BF16, st), f"B_v{i}") for i in range(2)])
        esel = sb("B_esel", [35, NB * 128], BF16, st)
        qrow = sb("B_qrow", [35, NHC * 512], BF16, st)
        caus = sb("B_caus", [128, 2 * 4 * 512], BF16, st)
        bcol = sb("B_bcol", [128, NHC * 64], F32, st)
        rw = sb("B_rw", [128, 3 * 64], F32, st)
        for nm, d, s_ in (("esel", esel, esel_d), ("qrow", qrow, qrow_d), ("caus", caus, caus_d), ("bcol", bcol, bcol_d), ("rw", rw, rw_d)):
            P.op('sp', (lambda e, d=d, s_=s_: e.dma_start(out=d[:], in_=s_)), writes=["B_" + nm], dma="B_" + nm)
        kmr = Ring([(sb(f"B_km{i}", [128, NB], BF16, st), f"B_km{i}") for i in range(2)])
        kmf = sb("B_kmf", [128, NB], F32, st)
        gmr = Ring([(sb(f"B_gm{i}", [128, 4, 32], F32, st), f"B_gm{i}") for i in range(2)])
        mx8 = sb("B_mx8", [128, 4, 8], F32, st)
        mbr = Ring([(sb(f"B_mbb{i}", [128, 4, 32], BF16, st), f"B_mbb{i}") for i in range(2)])
        rhx = Ring([(sb(f"B_rhx{i}", [35, 512], BF16, st), f"B_rhx{i}") for i in range(2)])
        for rx_, rxn_ in rhx.items:
            P.op('dve', (lambda e, rx_=rx_: e.memset(rx_[0:35, :], 0.0)), writes=[rxn_])
        wk = {nm: Ring([(sb(f"B_{nm}{i}", [128, 512], BF16, st), f"B_{nm}{i}") for i in range(6)]) for nm in ("e", "sp", "e2", "a", "p")}
        ystg = Ring([(sb(f"B_y{i}", [128, 512], BF16, st), f"B_y{i}") for i in range(2)])
        onesb = sb("B_onesb", [128, 1], F32, st)
        P.op('dve', (lambda e: e.memset(onesb[:, :], 1.0)), writes=['onesb'])
        GW_ = max(NB, 8)

        P.op('act', (lambda e: e.dma_start(out=MQK, in_=SQK[:, :, rk_act, :, :, :].rearrange("q th o hh d t -> q th (o hh) d t"))),
             reads=['SQKV'], writes=['MQKV'], dma='xq')
        P.op('act', (lambda e: e.dma_start(out=MV, in_=SV[:, :, :, rk_act, :].rearrange("v th t o c -> v th t (o c)"))),
             reads=['SQKV'], writes=['MQKV'], dma='xv')

        def load_head(hh, Kd, Qd, Vd):
            kT, kTn = kTr.next()
            qT, qTn = qTr.next()
            v, vn = vr.next()
            ksrc = MQK[Kd, :, hh, :, :].rearrange("th d t -> d th t")
            qsrc = MQK[Qd, :, hh, :, :].rearrange("th d t -> d th t")
            P.op('sp', (lambda e: e.dma_start(out=kT[:, :].rearrange("p (th t) -> p th t", th=2), in_=ksrc)), reads=['MQKV'], writes=[kTn], dma=kTn)
            P.op('sp', (lambda e: e.dma_start(out=qT[:, :].rearrange("p (th t) -> p th t", th=2), in_=qsrc)), reads=['MQKV'], writes=[qTn], dma=qTn)
            vsrc = MV[Vd, :, :, hh * 128:(hh + 1) * 128].rearrange("th (kt p) d -> p (th kt) d", p=128)
            P.op('sp', (lambda e: e.dma_start(out=v[:, :, :], in_=vsrc)), reads=['MQKV'], writes=[vn], dma=vn)
            return kT, kTn, qT, qTn, v, vn

        def moba_sel(qi, ts, gm, gmn, pg, mbb, mbn):
            own = 2 * qi + ts // 2
            w0 = 32 - own
            P.op('dve', (lambda e: e.tensor_tensor(out=gm[:, ts, 0:NB], in0=pg[0][:, ts * 32:ts * 32 + NB], in1=rw[:, w0:w0 + NB], op=ALU.add)),
                 reads=[pg[1], 'B_rw'], writes=[gmn])
            P.op('dve', (lambda e: e.max(out=mx8[:, ts, :], in_=gm[:, ts, 0:GW_])), reads=[gmn], writes=['B_mx8'])
            P.op('dve', (lambda e: e.tensor_scalar(out=gm[:, ts, 0:NB], in0=gm[:, ts, 0:NB], scalar1=mx8[:, ts, 2:3], scalar2=None, op0=ALU.is_ge)),
                 reads=['B_mx8', gmn], writes=[gmn])
            P.op('dve', (lambda e: e.tensor_tensor(out=gm[:, ts, 0:NB], in0=gm[:, ts, 0:NB], in1=rw[:, 64 + w0:64 + w0 + NB], op=ALU.mult)),
                 reads=[gmn, 'B_rw'], writes=[gmn])
            P.op('dve', (lambda e: e.tensor_tensor(out=gm[:, ts, 0:NB], in0=gm[:, ts, 0:NB], in1=rw[:, 128 + w0:128 + w0 + NB], op=ALU.add)),
                 reads=[gmn, 'B_rw'], writes=[gmn])
            P.op('dve', (lambda e: e.tensor_scalar(out=mbb[:, ts, 0:NB], in0=gm[:, ts, 0:NB], scalar1=-1.0, scalar2=-NEGM, op0=ALU.add, op1=ALU.mult)),
                 reads=[gmn], writes=[mbn])

        pgate = (ps[7][:, 0:128], 'ps7g')
        ptT = ps[7][:, :].bitcast(BF16)[:, 512:1024]

        def moba_smm(hh, qi, kt, kT, kTn, qT, qTn, rx, rxn, sb_):
            t0 = qi * 512
            diag = kt >= 4 * qi
            n = kt // 2
            dd = 4 * qi - kt + 3
            j = kt - 4 * qi

            def smm(e):
                e.matmul(sb_[0][:, :], kT[:, kt * 128:(kt + 1) * 128], qT[:, t0:t0 + 512], start=True, stop=False)
                if diag:
                    e.matmul(sb_[0][:, :], IDENT, caus[:, j * 512:(j + 1) * 512], start=False, stop=False)
                return e.matmul(sb_[0][:, :], esel[0:35, n * 128:(n + 1) * 128], rx[0:35, :], start=False, stop=True)
            P.op('pe', smm, reads=[kTn, qTn, rxn, 'B_esel', 'B_caus', CONSTS[4]], writes=[sb_[1]])
            pt, ptn = wk['p'].next()
            P.op('act', (lambda e: e.activation(out=pt[:, :], in_=sb_[0][:, :], func=AF.Exp, scale=SCALE,
                                                bias=bcol[:, hh * 64 + dd:hh * 64 + dd + 1])),
                 reads=[sb_[1], 'B_bcol'], writes=[ptn])
            return pt, ptn

        def moba_pv(kt, nkt, v, vn, pt, ptn, pN, pD):
            def pv(e):
                e.matmul(pN[0][:, :], v[:, kt, :], pt[:, :], start=(kt == 0), stop=(kt == nkt - 1))
                return e.matmul(pD[0][:, :], ONES, pt[:, :], start=(kt == 0), stop=(kt == nkt - 1))
            P.op('pe', pv, reads=[vn, ptn, CONSTS[4]], writes=[pN[1], pD[1]])

        def moba_prep1(hh, qi, qT, qTn, km, kmn):
            t0 = qi * 512
            gm, gmn = gmr.next()
            mbb, mbn = mbr.next()
            pg = pgate

            def gmm(e):
                ins = None
                for ts in range(4):
                    ins = e.matmul(pg[0][:, ts * 32:ts * 32 + NB], qT[:, t0 + ts * 128:t0 + (ts + 1) * 128], km[:, :], start=True, stop=True)
                return ins
            P.op('pe', gmm, reads=[qTn, kmn], writes=[pg[1]])
            if NB < 8:
                P.op('dve', (lambda e: e.memset(gm[:, :, :], -3.0e9)), writes=[gmn])
            for ts in range(4):
                moba_sel(qi, ts, gm, gmn, pg, mbb, mbn)
            return mbb, mbn

        def moba_prep2(hh, mbb, mbn):
            rx, rxn = rhx.next()

            def tr(e):
                ins = None
                for ts in range(4):
                    ins = e.transpose(ptT[0:NB, ts * 128:(ts + 1) * 128], mbb[:, ts, 0:NB], IDENT)
                return ins
            P.op('pe', tr, reads=[mbn, CONSTS[4]], writes=['ps7t'])
            P.op('act', (lambda e: e.copy(out=rx[0:NB, :], in_=ptT[0:NB, 0:512])), reads=['ps7t'], writes=[rxn])
            P.op('pool', (lambda e: e.tensor_copy(out=rx[32:35, :], in_=qrow[32:35, hh * 512:(hh + 1) * 512])), reads=['B_qrow'], writes=[rxn])
            return rx, rxn

        def moba_units(hh, qi, kT, kTn, qT, qTn, v, vn, rx, rxn, pN, pD, between=None):
            t0 = qi * 512
            nkt = 4 * qi + 4
            LA = 2
            pts = {}
            for s in range(nkt + LA):
                if s < nkt:
                    pts[s] = moba_smm(hh, qi, s, kT, kTn, qT, qTn, rx, rxn, psS.next())
                if s == 0 and between is not None:
                    between()
                if s - LA >= 0:
                    pt, ptn = pts.pop(s - LA)
                    moba_pv(s - LA, nkt, v, vn, pt, ptn, pN, pD)
            ys, ysn = ystg.next()
            rc, rcn = rcr.next()
            P.op('dve', (lambda e: e.reciprocal(out=rc[:, :], in_=pD[0][:, :])), reads=[pD[1]], writes=[rcn])
            P.op('dve', (lambda e: e.tensor_tensor(out=ys[:, :], in0=pN[0][:, :], in1=rc[:, :], op=ALU.mult)), reads=[pN[1], rcn], writes=[ysn])
            dst = LY[0, hh * 128:(hh + 1) * 128, t0 // TC, (t0 % TC):(t0 % TC) + 512]
            P.op('sp', (lambda e: e.dma_start(out=dst, in_=ys[:, :])), reads=[ysn], writes=['Y'], dma=ysn)

        def moba_head(hh):
            kT, kTn, qT, qTn, v, vn = load_head(hh, 1, 0, 0)
            km, kmn = kmr.next()
            P.op('dve', (lambda e: e.tensor_reduce(out=kmf[:, :], in_=kT[:, :].rearrange("p (n k) -> p n k", k=256), axis=AX.X, op=ALU.add)),
                 reads=[kTn], writes=['B_kmf'])
            P.op('dve', (lambda e: e.tensor_scalar(out=km[:, :], in0=kmf[:, :], scalar1=1.0 / 256, scalar2=None, op0=ALU.mult)),
                 reads=['B_kmf'], writes=[kmn])
            nxt = moba_prep1(hh, 0, qT, qTn, km, kmn)
            for qi in range(NQT):
                rx, rxn = moba_prep2(hh, *nxt)
                holder = {}

                def between(qi=qi):
                    if qi + 1 < NQT:
                        holder['n'] = moba_prep1(hh, qi + 1, qT, qTn, km, kmn)
                pN, pD = psND.next()
                moba_units(hh, qi, kT, kTn, qT, qTn, v, vn, rx, rxn, pN, pD, between)
                nxt = holder.get('n')

        def sb_z(qi, kt, kT, kTn, qT, qTn, zb):
            t0 = qi * 512
            diag = kt >= 4 * qi
            j = kt - 4 * qi

            def zmm(e):
                ins = e.matmul(zb[0][:, :], kT[:, kt * 128:(kt + 1) * 128], qT[:, t0:t0 + 512], start=True, stop=not diag)
                if diag:
                    ins = e.matmul(zb[0][:, :], IDENT, caus[:, (4 + j) * 512:(5 + j) * 512], start=False, stop=True)
                return ins
            P.op('pe', zmm, reads=[kTn, qTn, 'B_caus', CONSTS[4]], writes=[zb[1]])

        def sb_e(zb):
            et, etn = wk['e'].next()
            spt, sptn = wk['sp'].next()
            P.op('act', (lambda e: e.activation(out=et[:, :], in_=zb[0][:, :], func=AF.Exp)), reads=[zb[1]], writes=[etn])
            P.op('act', (lambda e: e.activation(out=spt[:, :], in_=et[:, :], func=AF.Ln, bias=onesb[:, 0:1])), reads=[etn, 'onesb'], writes=[sptn])
            return et, etn, spt, sptn

        def sb_tri(i, spt, sptn, pR):
            P.op('pe', (lambda e: e.matmul(pR[0][:, :], TRI, spt[:, :], start=(i == 0), stop=False, skip_group_check=True)),
                 reads=[sptn, CONSTS[4]], writes=[pR[1]])
            e2, e2n = wk['e2'].next()
            P.op('act', (lambda e: e.activation(out=e2[:, :], in_=pR[0][:, :], func=AF.Exp, scale=-1.0)), reads=[pR[1]], writes=[e2n])
            return e2, e2n

        def sb_ntri(i, nkt, spt, sptn, e2n, pR):
            P.op('pe', (lambda e: e.matmul(pR[0][:, :], NTRI, spt[:, :], start=False, stop=(i == nkt - 1), skip_group_check=True)),
                 reads=[sptn, e2n, CONSTS[4]], writes=[pR[1]])

        def sb_pv(i, nkt, kt, et, etn, e2, e2n, v, vn, pO):
            at, atn = wk['a'].next()
            P.op('dve', (lambda e: e.tensor_tensor(out=at[:, :], in0=et[:, :], in1=e2[:, :], op=ALU.mult)), reads=[etn, e2n], writes=[atn])
            P.op('pe', (lambda e: e.matmul(pO[0][:, :], v[:, kt, :], at[:, :], start=(i == 0), stop=(i == nkt - 1))),
                 reads=[vn, atn], writes=[pO[1]])

        def sb_qtile(hh, qi, kT, kTn, qT, qTn, v, vn, pR, pO):
            t0 = qi * 512
            nkt = 4 * qi + 4
            kts = list(range(nkt - 1, -1, -1))
            zbs, es, e2s = {}, {}, {}
            for s in range(nkt + 5):
                i = s - 3
                if 0 <= i - 1 < nkt:
                    et, etn, spt, sptn = es[i - 1]
                    sb_ntri(i - 1, nkt, spt, sptn, e2s[i - 1][1], pR)
                if 0 <= i < nkt:
                    et, etn, spt, sptn = es[i]
                    e2s[i] = sb_tri(i, spt, sptn, pR)
                if 0 <= i - 1 < nkt:
                    et, etn, spt, sptn = es.pop(i - 1)
                    e2, e2n = e2s.pop(i - 1)
                    sb_pv(i - 1, nkt, kts[i - 1], et, etn, e2, e2n, v, vn, pO)
                if s < nkt:
                    zbs[s] = psZ.next()
                    sb_z(qi, kts[s], kT, kTn, qT, qTn, zbs[s])
                j = s - 1
                if 0 <= j < nkt:
                    es[j] = sb_e(zbs.pop(j))
            ys, ysn = ystg.next()
            P.op('act', (lambda e: e.copy(out=ys[:, :], in_=pO[0][:, :])), reads=[pO[1]], writes=[ysn])
            dst = LY[1, hh * 128:(hh + 1) * 128, t0 // TC, (t0 % TC):(t0 % TC) + 512]
            P.op('sp', (lambda e: e.dma_start(out=dst, in_=ys[:, :])), reads=[ysn], writes=['Y'], dma=ysn)

        def sb_head(hh):
            kT, kTn, qT, qTn, v, vn = load_head(hh, 3, 2, 1)
            for qi in range(NQT):
                sb_qtile(hh, qi, kT, kTn, qT, qTn, v, vn, psR.next(), psO.next())

        psS = Ring([(ps[i], f"ps{i}") for i in range(3)])
        psND = Ring([((ps[3], 'ps3'), (ps[4], 'ps4')), ((ps[5], 'ps5'), (ps[6], 'ps6'))])
        rcr = Ring([(sb(f"B_rcp{i}", [128, 512], F32, st), f"B_rcp{i}") for i in range(2)])
        psZ = Ring([(ps[i], f"ps{i}") for i in range(4)])
        psR = Ring([(ps[4], 'ps4'), (ps[5], 'ps5')])
        psO = Ring([(ps[6], 'ps6'), (ps[7], 'ps7')])
        if 'moba' in PARTS:
            for hh in range(NHC):
                moba_head(hh)
            P.flush()
        if 'sb' in PARTS:
            for hh in range(NHC):
                sb_head(hh)
        P.op('act', (lambda e: e.dma_start(out=SY[:, rk_act, :, :, :].rearrange("b o f th t -> b (o f) th t"), in_=LY)), reads=['Y'], writes=['SY'], dma='xy')
        P.flush()
        st.close()

    def phase_C(l, xsrc, xdst):
        st = ExitStack()
        xt = sb("C_xt", [128, KC, 512], F32, st)
        A1 = sb("C_a1", [128, max(KC, 2 * KCW), 512], BF16, st)
        A2 = sb("C_a2", [128, max(KC, FCH // 128), 512], BF16, st)
        KW = max(KC, KCW, FCH // 128)
        wr = Ring([(sb(f"C_w{i}", [128, KW, 256], BF16, st), f"C_w{i}") for i in range(2)])
        sqr = Ring([(sb(f"C_sq{i}", [128, 4, 512], F32, st), f"C_sq{i}") for i in range(2)])
        rtr = Ring([(sb(f"C_rt{i}", [128, 512], F32, st), f"C_rt{i}") for i in range(2)])
        gr = Ring([(sb(f"C_g{i}", [128, 512], BF16, st), f"C_g{i}") for i in range(3)])
        tmr = Ring([(sb(f"C_t{i}", [128, 512], F32, st), f"C_t{i}") for i in range(3)])
        psg = Ring([(ps[i], f"ps{i}") for i in range(6)])
        psn = Ring([(ps[i], f"ps{i}") for i in (6, 7)])
        ur = Ring([(A2, 'C_a2')])
        P.op('act', (lambda e: e.dma_start(out=CY, in_=SY[:, :, :, rk_act, :].rearrange("b hr f o t -> b hr f (o t)"))), reads=['SY'], writes=['CY'], dma='xy')
        for tp in range(NT):
            t0 = tp * 512
            src = xsrc[:, t0:t0 + 512].rearrange("(kc p) t -> p kc t", p=128)
            P.op('sp', (lambda e, src=src: e.dma_start(out=xt[:, :, :], in_=src)), reads=['X'], writes=['C_xt'], dma='C_xt')
            ysa = CY[0, :, :, t0:t0 + 512].rearrange("hr (kc p) t -> p (hr kc) t", p=128)
            ysb = CY[1, :, :, t0:t0 + 512].rearrange("hr (kc p) t -> p (hr kc) t", p=128)
            P.op('sp', (lambda e, ysa=ysa: e.dma_start(out=A1[:, 0:KCW, :], in_=ysa)), reads=['CY'], writes=['C_a1'], dma='C_a1a')
            P.op('sp', (lambda e, ysb=ysb: e.dma_start(out=A1[:, KCW:2 * KCW, :], in_=ysb)), reads=['CY'], writes=['C_a1'], dma='C_a1b')

            def epi1(mt, pb, t0=t0):
                g, gn = gr.next()
                gsrc = G[mt * 128:(mt + 1) * 128, t0:t0 + 512]
                P.op('sp', (lambda e: e.dma_start(out=g[:, :], in_=gsrc)), reads=['G'], writes=[gn], dma=gn)
                P.op('dve', (lambda e: e.tensor_tensor(out=A2[:, mt, :], in0=pb[0][:, :], in1=g[:, :], op=ALU.mult)), reads=[pb[1], gn], writes=['C_a2'])
            gemm(A1[:, 0:KCW, :], 'C_a1', KCW, wb[("wbm", l)], f"wb_wbm{l}", 0, 0, D, 256, wr, psg, epi1)

            def epi2(mt, pb, t0=t0):
                g, gn = gr.next()
                gsrc = G[D + mt * 128:D + (mt + 1) * 128, t0:t0 + 512]
                P.op('sp', (lambda e: e.dma_start(out=g[:, :], in_=gsrc)), reads=['G'], writes=[gn], dma=gn)
                tm, tmn = tmr.next()
                P.op('dve', (lambda e: e.tensor_tensor(out=tm[:, :], in0=pb[0][:, :], in1=g[:, :], op=ALU.mult)), reads=[pb[1], gn], writes=[tmn])
                P.op('pool', (lambda e: e.tensor_tensor(out=A2[:, mt, :], in0=A2[:, mt, :], in1=tm[:, :], op=ALU.add)), reads=[tmn, 'C_a2'], writes=['C_a2'])
            gemm(A1[:, KCW:2 * KCW, :], 'C_a1', KCW, wb[("wbs", l)], f"wb_wbs{l}", 0, 0, D, 256, wr, psg, epi2)

            def epi3(mt, pb):
                P.op('dve', (lambda e: e.tensor_tensor(out=xt[:, mt, :], in0=xt[:, mt, :], in1=pb[0][:, :], op=ALU.add)), reads=[pb[1], 'C_xt'], writes=['C_xt'])
            gemm(A2[:, 0:KC, :], 'C_a2', KC, wb[("wout", l)], f"wb_wout{l}", 0, 0, D, 256, wr, psg, epi3)
            rmsnorm(xt, A1, gmlp, l * KC, sqr, rtr, psn, 'C_xt', 'C_a1')
            for fc in range(NFC):
                U, Un = ur.next()

                def epi4(mt, pb, U=U, Un=Un):
                    tm, tmn = tmr.next()
                    P.op('act', (lambda e: e.activation(out=tm[:, :], in_=pb[0][:, :], func=AF.Relu)), reads=[pb[1]], writes=[tmn])
                    P.op('pool', (lambda e: e.tensor_tensor(out=U[:, mt, :], in0=tm[:, :], in1=tm[:, :], op=ALU.mult)), reads=[tmn], writes=[Un])
                gemm(A1[:, 0:KC, :], 'C_a1', KC, wb[("wup", l)], f"wb_wup{l}", 0, fc * FCH, FCH, 256, wr, psg, epi4)
                gemm(U[:, 0:FCH // 128, :], Un, FCH // 128, wb[("wdn", l)], f"wb_wdn{l}", fc * FCH, 0, D, 256, wr, psg, epi3)
            dst = xdst[:, t0:t0 + 512].rearrange("(kc p) t -> p kc t", p=128)
            P.op('sp', (lambda e, dst=dst: e.dma_start(out=dst, in_=xt[:, :, :])), reads=['C_xt'], writes=['X'], dma='C_xst')
        P.flush()
        st.close()

    dbg_out = {}
    for l in range(NL):
        xsrc = xT if l == 0 else X1
        xdst = X1 if l < NL - 1 else outT
        if 'A' in PARTS:
            phase_A(l, xsrc)
        P.barrier()
        phase_B(l)
        P.barrier()
        if 'C' in PARTS:
            phase_C(l, xsrc, xdst)
        if dbg and l == dbg - 1:
            for nm, t in (("LQK", LQK), ("LV", LV), ("LY", LY), ("G", G)):
                o = nc.dram_tensor("dbg_" + nm, list(t.shape), BF16, kind="ExternalOutput").ap()
                P.op('sp', (lambda e, o=o, t=t: e.dma_start(out=o, in_=t)), reads=['QKV', 'Y', 'G'], writes=['dbg' + nm], dma='dbg' + nm)
            break
    P.flush()
    return nc


def _tables(cfg, r):
    import ml_dtypes
    bf = ml_dtypes.bfloat16
    NB, NHC, NH = cfg.NB, cfg.NHC, cfg.NH
    scale = 128.0 ** -0.5
    slopes_all = 2.0 ** (-8.0 * np.arange(1, NH + 1, dtype=np.float64) / NH)
    sl = slopes_all[r * NHC:(r + 1) * NHC]
    k = np.arange(128, dtype=np.float64)[:, None]
    dd = np.arange(64, dtype=np.float64)[None, :] - 3.0
    bcol = np.concatenate([(s * (k - 128.0 * dd)) for s in sl], axis=1).astype(np.float32)
    w = np.arange(64)
    rowneg = np.where(w < 32, 0.0, -1.0e9)
    rowpast = np.where(w < 32, 1.0, 0.0)
    rowown = np.where(w == 32, 1.0, 0.0)
    rw = np.tile(np.concatenate([rowneg, rowpast, rowown])[None, :], (128, 1)).astype(np.float32)
    esel = np.zeros((35, NB, 128), np.float32)
    for n in range(min(NB, 32)):
        esel[n, n, :] = 1.0
    esel[32:35, :, :] = 1.0
    esel = esel.reshape(35, NB * 128).astype(bf)
    qrow = np.zeros((35, NHC, 512), np.float32)
    i = np.arange(512, dtype=np.float64)
    for hh, s in enumerate(sl):
        val = -s * i / scale
        a = val.astype(np.float32).astype(bf)
        rem = val - a.astype(np.float64)
        b = rem.astype(np.float32).astype(bf)
        rem2 = rem - b.astype(np.float64)
        c = rem2.astype(np.float32).astype(bf)
        qrow[32, hh], qrow[33, hh], qrow[34, hh] = a.astype(np.float32), b.astype(np.float32), c.astype(np.float32)
    qrow = qrow.reshape(35, NHC * 512).astype(bf)
    caus = np.zeros((128, 2, 4, 512), np.float32)
    kk = np.arange(128)[:, None]
    tt = np.arange(512)[None, :]
    for j in range(4):
        kp = j * 128 + kk
        caus[:, 0, j, :] = np.where(kp <= tt, 0.0, NEGM)
        caus[:, 1, j, :] = np.where(kp < tt, 0.0, NEGM)
    caus = caus.reshape(128, 2 * 4 * 512).astype(bf)
    ident = np.eye(128, dtype=np.float32)
    tri = (np.arange(128)[:, None] >= np.arange(128)[None, :]).astype(np.float32)
    ntri = 1.0 - tri
    ones = np.ones((128, 128), np.float32)
    cst = np.concatenate([ident, tri, ntri, ones], axis=1).astype(bf)
    return dict(bcol=bcol, rw=rw, esel=esel, qrow=qrow, caus=caus, cst=cst, onesf=np.ones((128, 128), np.float32))


def _pvec(v, KCn):
    L = v.shape[0]
    return np.ascontiguousarray(v.reshape(L, KCn, 128).transpose(2, 0, 1).reshape(128, L * KCn)).astype(np.float32)


def make_in_maps(cfg, x, norm_mix, w_in, b_gate, q_norm, k_norm, w_branch_moba, w_branch_sb, w_out, norm_mlp, w_up, w_down):
    NL, D, TC = cfg.NL, cfg.D, cfg.TC
    ws = {"win": w_in, "wbm": w_branch_moba, "wbs": w_branch_sb, "wout": w_out, "wup": w_up, "wdn": w_down}
    tabs = [_tables(cfg, r) for r in range(2)]
    gq = np.stack([np.asarray(q_norm), np.asarray(k_norm)], axis=1)
    gqk = np.ascontiguousarray(gq.transpose(2, 0, 1).reshape(128, NL * 2)).astype(np.float32)
    common = dict(gmix=_pvec(np.asarray(norm_mix), cfg.KC), gmlp=_pvec(np.asarray(norm_mlp), cfg.KC),
                  bgate=_pvec(np.asarray(b_gate), 2 * cfg.KC), gqk=gqk)
    halves = {}
    for nm, w in ws.items():
        w = np.asarray(w)
        Kh = w.shape[1] // 2
        for l in range(NL):
            for r in range(2):
                halves[(nm, l, r)] = np.ascontiguousarray(w[l, r * Kh:(r + 1) * Kh, :])
    xTs = {}
    for b in range(2):
        for r in range(2):
            xTs[(b, r)] = np.ascontiguousarray(np.asarray(x)[b, r * TC:(r + 1) * TC, :].T)
    epoch = int(np.random.randint(1, 1 << 24))
    common["barv"] = (epoch * 16 + np.arange(16)).astype(np.int32)[None, :]
    in_maps = []
    for c in range(8):
        b, r = (c // 2) % 2, c % 2
        m = dict(common)
        m.update(tabs[r])
        m["xT"] = xTs[(b, r)]
        for nm in ws:
            for l in range(NL):
                m[f"{nm}{l}"] = halves[(nm, l, r)]
        in_maps.append(m)
    return in_maps


_CACHE = {}


def kernel(x, norm_mix, w_in, b_gate, q_norm, k_norm, w_branch_moba, w_branch_sb, w_out, norm_mlp, w_up, w_down):
    cfg = Cfg()
    if 'nc' not in _CACHE:
        _CACHE['nc'] = build(cfg)
    nc = _CACHE['nc']
    in_maps = make_in_maps(cfg, x, norm_mix, w_in, b_gate, q_norm, k_norm, w_branch_moba, w_branch_sb, w_out, norm_mlp, w_up, w_down)
    res = run_bass_kernel_spmd(nc, in_maps, core_ids=list(range(8)))
    out = np.empty((2, cfg.S, cfg.D), np.float32)
    for c in range(4):
        b, r = c // 2, c % 2
        out[b, r * cfg.TC:(r + 1) * cfg.TC, :] = res.results[c]["outT"].T
    return out
```

```python
import numpy as np
from contextlib import ExitStack
import concourse.bass as bass
import concourse.mybir as mybir
from concourse.bass_utils import run_bass_kernel_spmd

F32 = mybir.dt.float32
BF16 = mybir.dt.bfloat16
AF = mybir.ActivationFunctionType
ALU = mybir.AluOpType
AX = mybir.AxisListType
ENG = ('pe', 'act', 'dve', 'pool', 'sp')
EPS = 1e-6
NEGM = -30000.0


class Cfg:
    def __init__(self, D=4096, NH=16, S=8192, DFF=16384, NL=2):
        self.D, self.NH, self.S, self.DFF, self.NL = D, NH, S, DFF, NL
        self.TC = S // 2
        self.KC = D // 128
        self.WD = NH * 128
        self.KCW = NH
        self.INC = 6 * self.WD + 2 * D
        self.NB = S // 256
        self.NT = self.TC // 512
        self.NHC = NH // 2
        self.NQT = S // 512
        self.NKT = S // 128
        self.FCH = min(DFF, 4096)
        self.NFC = DFF // self.FCH


class Prog:
    def __init__(self, nc, stack):
        self.nc, self.stack = nc, stack
        self.sems, self.count = {}, {}
        self.ops = {e: [] for e in ENG}
        self.waited = {e: {} for e in ENG}
        self.Wt, self.Rd = {}, {}
        self.nbar = 0
        for e in ('pe', 'act', 'dve', 'pool'):
            self._sem('e_' + e)

    def _sem(self, name):
        if name not in self.sems:
            self.sems[name] = self.stack.enter_context(self.nc.semaphore(name))
            self.count[name] = 0
        return self.sems[name]

    def op(self, eng, emit, reads=(), writes=(), dma=None):
        deps = {}

        def add(d):
            for s, v in d.items():
                if deps.get(s, 0) < v:
                    deps[s] = v
        for r in reads:
            add(self.Wt.get(r, {}))
        for w in writes:
            add(self.Wt.get(w, {}))
            add(self.Rd.get(w, {}))
        if dma is not None:
            sname = 'd_' + dma
            self._sem(sname)
            if self.count[sname] > 0:
                add({sname: self.count[sname]})
            inc = 16
        else:
            sname = 'e_' + eng
            inc = 1
        self.count[sname] += inc
        tok = self.count[sname]
        waits = []
        wd = self.waited[eng]
        for s, v in deps.items():
            if eng == 'pe' and s == 'e_pe':
                continue
            if wd.get(s, 0) >= v:
                continue
            wd[s] = v
            waits.append((s, v))
        self.ops[eng].append((waits, emit, sname, inc))
        for r in reads:
            d = self.Rd.setdefault(r, {})
            d[sname] = max(d.get(sname, 0), tok)
        for w in writes:
            d = self.Wt.setdefault(w, {})
            d[sname] = max(d.get(sname, 0), tok)

    def flush(self):
        nc = self.nc
        endw = [(s, v) for s, v in self.count.items() if v > 0]
        ops = self.ops
        sems = self.sems
        waited = self.waited

        def mk(en):
            def f(e):
                for waits, emit, sname, inc in ops[en]:
                    for s, v in waits:
                        e.wait_ge(sems[s], v)
                    ins = emit(e)
                    ins.then_inc(sems[sname], inc)
                for s, v in endw:
                    if waited[en].get(s, 0) < v:
                        e.wait_ge(sems[s], v)
                        waited[en][s] = v
            return f
        with nc.Block() as block:
            block.tensor(mk('pe'))
            block.scalar(mk('act'))
            block.vector(mk('dve'))
            block.gpsimd(mk('pool'))
            block.sync(mk('sp'))
        self.ops = {e: [] for e in ENG}

    def barrier(self):
        self.flush()
        nc = self.nc
        self.nbar += 1
        k = self.nbar
        flags, barv, rk = self.flags, self.barv, self.rk_sp
        sdma, sdone = self._sem('bar_dma'), self._sem('bar_done')
        with nc.Block() as block:
            def spf(e):
                e.dma_start(out=flags[rk, 0:1].rearrange("o c -> (o c)"), in_=barv[0, k:k + 1]).then_inc(sdma, 16)
                e.wait_ge(sdma, 16 * k)
                with e.register(f"bv{k}") as v, e.register(f"bf0{k}") as f0, e.register(f"bf1{k}") as f1, e.register(f"bc{k}") as c:
                    e.load(v, barv[0:1, k:k + 1])
                    e.reg_mov(c, 1)
                    with e.While(c):
                        e.load(f0, flags[0:1, 0:1])
                        e.load(f1, flags[1:2, 0:1])
                        e.reg_alu(f0, f0, v, ALU.bitwise_xor)
                        e.reg_alu(f1, f1, v, ALU.bitwise_xor)
                        e.reg_alu(c, f0, f1, ALU.bitwise_or)
                e.sem_inc(sdone, 1)

            def other(e):
                e.wait_ge(sdone, k)
            block.sync(spf)
            block.tensor(other)
            block.scalar(other)
            block.vector(other)
            block.gpsimd(other)


class Ring:
    def __init__(self, items):
        self.items, self.i = list(items), 0

    def next(self):
        it = self.items[self.i % len(self.items)]
        self.i += 1
        return it


def build(cfg, PARTS=('A', 'moba', 'sb', 'C'), dbg=False):
    D, NH, S, DFF, NL = cfg.D, cfg.NH, cfg.S, cfg.DFF, cfg.NL
    TC, KC, WD, KCW, INC, NB, NT, NHC = cfg.TC, cfg.KC, cfg.WD, cfg.KCW, cfg.INC, cfg.NB, cfg.NT, cfg.NHC
    NQT, NKT, FCH, NFC = cfg.NQT, cfg.NKT, cfg.FCH, cfg.NFC
    SCALE = 128.0 ** -0.5
    nc = bass.Bass("TRN2", target_bir_lowering=False, num_devices=8)
    stack = ExitStack()
    P = Prog(nc, stack)

    def din(name, shape, dt=F32):
        return nc.dram_tensor(name, list(shape), dt, kind="ExternalInput").ap()

    def dint(name, shape, dt, shared=False):
        return nc.dram_tensor(name, list(shape), dt, addr_space=("Shared" if shared else "Local")).ap()

    xT = din("xT", [D, TC])
    wnames = [("win", D, INC), ("wbm", WD, D), ("wbs", WD, D), ("wout", D, D), ("wup", D, DFF), ("wdn", DFF, D)]
    w_in32 = {}
    for l in range(NL):
        for nm, K, M in wnames:
            w_in32[(nm, l)] = din(f"{nm}{l}", [K // 2, M])
    gmix_d = din("gmix", [128, NL * KC])
    gmlp_d = din("gmlp", [128, NL * KC])
    bgate_d = din("bgate", [128, NL * 2 * KC])
    gqk_d = din("gqk", [128, NL * 2])
    bcol_d = din("bcol", [128, NHC * 64])
    rw_d = din("rw", [128, 3 * 64])
    esel_d = din("esel", [35, NB * 128], BF16)
    qrow_d = din("qrow", [35, NHC * 512], BF16)
    caus_d = din("caus", [128, 2 * 4 * 512], BF16)
    cst_d = din("cst", [128, 4 * 128], BF16)
    onesf_d = din("onesf", [128, 128])
    outT = nc.dram_tensor("outT", [D, TC], F32, kind="ExternalOutput").ap()

    wb = {}
    for l in range(NL):
        for nm, K, M in wnames:
            wb[(nm, l)] = dint(f"wb_{nm}{l}", [2, K // 2, M], BF16, shared=True)
    SQK = dint("SQK", [4, 2, 2, NHC, 128, TC], BF16, True)
    SV = dint("SV", [2, 2, TC, 2, NHC * 128], BF16, True)
    SY = dint("SY", [2, 2, NHC * 128, 2, TC], BF16, True)
    LQK = dint("LQK", [4, NH, 128, TC], BF16)
    LV = dint("LV", [2, TC, WD], BF16)
    MQK = dint("MQK", [4, 2, NHC, 128, TC], BF16)
    MV = dint("MV", [2, 2, TC, NHC * 128], BF16)
    LY = dint("LY", [2, NHC * 128, 2, TC], BF16)
    CY = dint("CY", [2, 2, NHC * 128, TC], BF16)
    G = dint("G", [2 * D, TC], BF16)
    X1 = dint("X1", [D, TC], F32)

    pid = nc.gpsimd.partition_id()
    rk_pool = bass.ds(pid % 2, 1)
    rk_sp = bass.ds(nc.sync.partition_id() % 2, 1)
    rk_act = bass.ds(nc.scalar.partition_id() % 2, 1)
    I32 = mybir.dt.int32
    P.flags = nc.dram_tensor("barflags", [2, 16], I32, addr_space="Shared").ap()
    P.barv = nc.dram_tensor("barv", [1, 16], I32, kind="ExternalInput").ap()
    P.rk_sp = rk_sp

    _uid = [0]

    def sb(name, shape, dt, st=stack):
        _uid[0] += 1
        return st.enter_context(nc.sbuf_tensor(f"{name}_{_uid[0]}", list(shape), dt))

    gmix = sb("gmix_s", [128, NL * KC], F32)
    gmlp = sb("gmlp_s", [128, NL * KC], F32)
    bgate = sb("bgate_s", [128, NL * 2 * KC], F32)
    gqk = sb("gqk_s", [128, NL * 2], F32)
    cst = sb("cst_s", [128, 4 * 128], BF16)
    onesf = sb("onesf_s", [128, 128], F32)
    IDENT, TRI, NTRI, ONES = (cst[:, i * 128:(i + 1) * 128] for i in range(4))

    ps = [stack.enter_context(nc.psum_tensor(f"ps{i}", [128, 512], F32)) for i in range(8)]

    for i, (dst, src) in enumerate([(gmix, gmix_d), (gmlp, gmlp_d), (bgate, bgate_d), (gqk, gqk_d), (cst, cst_d), (onesf, onesf_d)]):
        P.op('sp', (lambda e, d=dst, s=src: e.dma_start(out=d[:], in_=s)), writes=[f"const{i}"], dma=f"const{i}")
    CONSTS = [f"const{i}" for i in range(6)]

    wi = 0
    for l in range(NL):
        for nm, K, M in wnames:
            src = w_in32[(nm, l)]
            dst = wb[(nm, l)][rk_pool, :, :].rearrange("o k m -> (o k) m")
            key = f"wc{wi % 4}"
            wi += 1
            P.op('pool', (lambda e, d=dst, s=src: e.dma_start(out=d, in_=s)), writes=[f"wb_{nm}{l}"], dma=key)
    P.barrier()

    def rmsnorm(xt, ht, gtab, goff, sqr, rtr, psn, tagx, tagh):
        pn = psn.next()
        G4 = 4 if KC % 4 == 0 else (2 if KC % 2 == 0 else 1)
        ngr = KC // G4
        for g in range(ngr):
            sq, sqn = sqr.next()
            P.op('act', (lambda e, sq=sq, g=g: e.activation(out=sq[:, 0:G4, :], in_=xt[:, g * G4:(g + 1) * G4, :], func=AF.Square)),
                 reads=[tagx], writes=[sqn])

            def mm(e, sq=sq, g=g, pn=pn):
                ins = None
                for j in range(G4):
                    ins = e.matmul(pn[0][:, :], onesf[:, :], sq[:, j, :], start=(g == 0 and j == 0), stop=(g == ngr - 1 and j == G4 - 1))
                return ins
            P.op('pe', mm, reads=[sqn, CONSTS[5]], writes=[pn[1]])
        rt, rtn = rtr.next()
        P.op('act', (lambda e: e.activation(out=rt[:, :], in_=pn[0][:, :], func=AF.Sqrt, scale=1.0 / D, bias=epsb[:, 0:1])),
             reads=[pn[1], 'epsb'], writes=[rtn])
        P.op('dve', (lambda e: e.reciprocal(out=rt[:, :], in_=rt[:, :])), reads=[rtn], writes=[rtn])
        for kc in range(KC):
            eng = 'dve'
            P.op(eng, (lambda e, kc=kc: e.scalar_tensor_tensor(out=ht[:, kc, :], in0=xt[:, kc, :], scalar=gtab[:, goff + kc:goff + kc + 1],
                                                                 in1=rt[:, :], op0=ALU.mult, op1=ALU.mult)),
                 reads=[tagx, rtn] + CONSTS[:2], writes=[tagh])

    def gemm(act, tagact, KCa, wdram, tagw, row0, col0, ncols, GW, wring, psring, epi, tokmajor=None):
        for g0 in range(0, ncols, GW):
            gw = min(GW, ncols - g0)
            wt, wtn = wring.next()
            src = wdram.rearrange("r k m -> (r k) m")[row0:row0 + KCa * 128, col0 + g0:col0 + g0 + gw].rearrange("(kc p) m -> p kc m", p=128)
            P.op('sp', (lambda e, wt=wt, src=src, gw=gw: e.dma_start(out=wt[:, 0:KCa, 0:gw], in_=src)),
                 reads=[tagw], writes=[wtn], dma=wtn)
            for m0 in range(0, gw, 128):
                mt = (g0 + m0) // 128
                pb = psring.next()
                if tokmajor is not None and tokmajor(mt):
                    def mm(e, wt=wt, m0=m0, pb=pb):
                        ins = None
                        for ts in range(4):
                            for kc in range(KCa):
                                ins = e.matmul(pb[0][:, ts * 128:(ts + 1) * 128], act[:, kc, ts * 128:(ts + 1) * 128], wt[:, kc, m0:m0 + 128],
                                               start=(kc == 0), stop=(kc == KCa - 1))
                        return ins
                else:
                    def mm(e, wt=wt, m0=m0, pb=pb):
                        ins = None
                        for kc in range(KCa):
                            ins = e.matmul(pb[0][:, :], wt[:, kc, m0:m0 + 128], act[:, kc, :], start=(kc == 0), stop=(kc == KCa - 1))
                        return ins
                P.op('pe', mm, reads=[wtn, tagact], writes=[pb[1]])
                epi(mt, pb)

    epsb = sb("epsb", [128, 1], F32)
    P.op('dve', (lambda e: e.memset(epsb[:, :], EPS)), writes=['epsb'])

    def phase_A(l, xsrc):
        st = ExitStack()
        xt = sb("A_xt", [128, KC, 512], F32, st)
        ht = sb("A_ht", [128, KC, 512], BF16, st)
        wr = Ring([(sb(f"A_w{i}", [128, KC, 256], BF16, st), f"A_w{i}") for i in range(4)])
        sqr = Ring([(sb(f"A_sq{i}", [128, 4, 512], F32, st), f"A_sq{i}") for i in range(2)])
        rtr = Ring([(sb(f"A_rt{i}", [128, 512], F32, st), f"A_rt{i}") for i in range(2)])
        sq1 = Ring([(sb(f"A_q{i}", [128, 512], F32, st), f"A_q{i}") for i in range(2)])
        stg = Ring([(sb(f"A_st{i}", [128, 512], BF16, st), f"A_st{i}") for i in range(4)])
        psg = Ring([(ps[i], f"ps{i}") for i in range(6)])
        psn = Ring([(ps[i], f"ps{i}") for i in (6, 7)])
        psq = psn
        segs = [("qa", WD), ("ka", WD), ("va", WD), ("qb", WD), ("kb", WD), ("vb", WD), ("ga", D), ("gb", D)]
        bounds = np.cumsum([0] + [s[1] for s in segs])

        def seg_of(mt):
            c = mt * 128
            i = int(np.searchsorted(bounds, c, side='right') - 1)
            return segs[i][0], (c - bounds[i]) // 128

        for tp in range(NT):
            t0 = tp * 512
            src = xsrc[:, t0:t0 + 512].rearrange("(kc p) t -> p kc t", p=128)
            P.op('sp', (lambda e, src=src: e.dma_start(out=xt[:, :, :], in_=src)), reads=['X'], writes=['A_xt'], dma='A_xt')
            rmsnorm(xt, ht, gmix, l * KC, sqr, rtr, psn, 'A_xt', 'A_ht')

            def epi(mt, pb, t0=t0):
                kind, j = seg_of(mt)
                so, son = stg.next()
                if kind in ('qa', 'ka'):
                    q1, q1n = sq1.next()
                    P.op('act', (lambda e: e.activation(out=q1[:, :], in_=pb[0][:, :], func=AF.Square)), reads=[pb[1]], writes=[q1n])
                    pq = psq.next()
                    P.op('pe', (lambda e: e.matmul(pq[0][:, :], onesf[:, :], q1[:, :], start=True, stop=True)), reads=[q1n, CONSTS[5]], writes=[pq[1]])
                    P.op('act', (lambda e: e.activation(out=q1[:, :], in_=pq[0][:, :], func=AF.Sqrt, scale=1.0 / 128, bias=epsb[:, 0:1])),
                         reads=[pq[1], 'epsb'], writes=[q1n])
                    P.op('dve', (lambda e: e.reciprocal(out=q1[:, :], in_=q1[:, :])), reads=[q1n], writes=[q1n])
                    gi = l * 2 + (0 if kind == 'qa' else 1)
                    P.op('dve', (lambda e: e.scalar_tensor_tensor(out=so[:, :], in0=pb[0][:, :], scalar=gqk[:, gi:gi + 1], in1=q1[:, :],
                                                                  op0=ALU.mult, op1=ALU.mult)),
                         reads=[pb[1], q1n, CONSTS[3]], writes=[son])
                    dst = LQK[0 if kind == 'qa' else 1, j, :, t0:t0 + 512]
                    P.op('pool', (lambda e: e.dma_start(out=dst, in_=so[:, :])), reads=[son], writes=['QKV'], dma=son)
                elif kind in ('qb', 'kb'):
                    sc = SCALE if kind == 'qb' else 1.0
                    P.op('act', (lambda e: e.activation(out=so[:, :], in_=pb[0][:, :], func=AF.Copy, scale=sc)), reads=[pb[1]], writes=[son])
                    dst = LQK[2 if kind == 'qb' else 3, j, :, t0:t0 + 512]
                    P.op('pool', (lambda e: e.dma_start(out=dst, in_=so[:, :])), reads=[son], writes=['QKV'], dma=son)
                elif kind in ('va', 'vb'):
                    P.op('dve', (lambda e: e.tensor_copy(out=so[:, :], in_=pb[0][:, :])), reads=[pb[1]], writes=[son])
                    dst = LV[0 if kind == 'va' else 1, t0:t0 + 512, j * 128:(j + 1) * 128].rearrange("(ts p) d -> p ts d", p=128)
                    P.op('pool', (lambda e: e.dma_start(out=dst, in_=so[:, :].rearrange("p (ts d) -> p ts d", d=128))),
                         reads=[son], writes=['QKV'], dma=son)
                else:
                    gofs = l * 2 * KC + (0 if kind == 'ga' else KC) + j
                    P.op('act', (lambda e: e.activation(out=so[:, :], in_=pb[0][:, :], func=AF.Sigmoid, bias=bgate[:, gofs:gofs + 1])),
                         reads=[pb[1], CONSTS[2]], writes=[son])
                    row = (0 if kind == 'ga' else D) + j * 128
                    dst = G[row:row + 128, t0:t0 + 512]
                    P.op('pool', (lambda e: e.dma_start(out=dst, in_=so[:, :])), reads=[son], writes=['G'], dma=son)

            gemm(ht, 'A_ht', KC, wb[("win", l)], f"wb_win{l}", 0, 0, INC, 256, wr, psg, epi,
                 tokmajor=lambda mt: seg_of(mt)[0] in ('va', 'vb'))
        P.op('act', (lambda e: e.dma_start(out=SQK[:, rk_act, :, :, :, :].rearrange("q o hr hh d t -> q (o hr) hh d t"),
                                          in_=LQK.rearrange("q (hr hh) d t -> q hr hh d t", hr=2))), reads=['QKV'], writes=['SQKV'], dma='xq')
        P.op('act', (lambda e: e.dma_start(out=SV[:, rk_act, :, :, :].rearrange("v o t hr c -> v (o t) hr c"),
                                          in_=LV.rearrange("v t (hr c) -> v t hr c", hr=2))), reads=['QKV'], writes=['SQKV'], dma='xv')
        P.flush()
        st.close()

    def phase_B(l):
        st = ExitStack()
        kTr = Ring([(sb(f"B_k{i}", [128, S], BF16, st), f"B_k{i}") for i in range(2)])
        qTr = Ring([(sb(f"B_q{i}", [128, S], BF16, st), f"B_q{i}") for i in range(2)])
        vr = Ring([(sb(f"B_v{i}", [128, NKT, 128], BF16, st), f"B_v{i}") for i in range(2)])
        esel = sb("B_esel", [35, NB * 128], BF16, st)
        qrow = sb("B_qrow", [35, NHC * 512], BF16, st)
        caus = sb("B_caus", [128, 2 * 4 * 512], BF16, st)
        bcol = sb("B_bcol", [128, NHC * 64], F32, st)
        rw = sb("B_rw", [128, 3 * 64], F32, st)
        for nm, d, s_ in (("esel", esel, esel_d), ("qrow", qrow, qrow_d), ("caus", caus, caus_d), ("bcol", bcol, bcol_d), ("rw", rw, rw_d)):
            P.op('sp', (lambda e, d=d, s_=s_: e.dma_start(out=d[:], in_=s_)), writes=["B_" + nm], dma="B_" + nm)
        kmr = Ring([(sb(f"B_km{i}", [128, NB], BF16, st), f"B_km{i}") for i in range(2)])
        kmf = sb("B_kmf", [128, NB], F32, st)
        gmr = Ring([(sb(f"B_gm{i}", [128, 4, 32], F32, st), f"B_gm{i}") for i in range(2)])
        mx8 = sb("B_mx8", [128, 4, 8], F32, st)
        mbr = Ring([(sb(f"B_mbb{i}", [128, 4, 32], BF16, st), f"B_mbb{i}") for i in range(2)])
        rhx = Ring([(sb(f"B_rhx{i}", [35, 512], BF16, st), f"B_rhx{i}") for i in range(2)])
        for rx_, rxn_ in rhx.items:
            P.op('dve', (lambda e, rx_=rx_: e.memset(rx_[0:35, :], 0.0)), writes=[rxn_])
        wk = {nm: Ring([(sb(f"B_{nm}{i}", [128, 512], BF16, st), f"B_{nm}{i}") for i in range(6)]) for nm in ("e", "sp", "e2", "a", "p")}
        ystg = Ring([(sb(f"B_y{i}", [128, 512], BF16, st), f"B_y{i}") for i in range(2)])
        onesb = sb("B_onesb", [128, 1], F32, st)
        P.op('dve', (lambda e: e.memset(onesb[:, :], 1.0)), writes=['onesb'])
        GW_ = max(NB, 8)

        P.op('act', (lambda e: e.dma_start(out=MQK, in_=SQK[:, :, rk_act, :, :, :].rearrange("q th o hh d t -> q th (o hh) d t"))),
             reads=['SQKV'], writes=['MQKV'], dma='xq')
        P.op('act', (lambda e: e.dma_start(out=MV, in_=SV[:, :, :, rk_act, :].rearrange("v th t o c -> v th t (o c)"))),
             reads=['SQKV'], writes=['MQKV'], dma='xv')

        def load_head(hh, Kd, Qd, Vd):
            kT, kTn = kTr.next()
            qT, qTn = qTr.next()
            v, vn = vr.next()
            ksrc = MQK[Kd, :, hh, :, :].rearrange("th d t -> d th t")
            qsrc = MQK[Qd, :, hh, :, :].rearrange("th d t -> d th t")
            P.op('sp', (lambda e: e.dma_start(out=kT[:, :].rearrange("p (th t) -> p th t", th=2), in_=ksrc)), reads=['MQKV'], writes=[kTn], dma=kTn)
            P.op('sp', (lambda e: e.dma_start(out=qT[:, :].rearrange("p (th t) -> p th t", th=2), in_=qsrc)), reads=['MQKV'], writes=[qTn], dma=qTn)
            vsrc = MV[Vd, :, :, hh * 128:(hh + 1) * 128].rearrange("th (kt p) d -> p (th kt) d", p=128)
            P.op('sp', (lambda e: e.dma_start(out=v[:, :, :], in_=vsrc)), reads=['MQKV'], writes=[vn], dma=vn)
            return kT, kTn, qT, qTn, v, vn

        def moba_sel(qi, ts, gm, gmn, pg, mbb, mbn):
            own = 2 * qi + ts // 2
            w0 = 32 - own
            P.op('dve', (lambda e: e.tensor_tensor(out=gm[:, ts, 0:NB], in0=pg[0][:, ts * 32:ts * 32 + NB], in1=rw[:, w0:w0 + NB], op=ALU.add)),
                 reads=[pg[1], 'B_rw'], writes=[gmn])
            P.op('dve', (lambda e: e.max(out=mx8[:, ts, :], in_=gm[:, ts, 0:GW_])), reads=[gmn], writes=['B_mx8'])
            P.op('dve', (lambda e: e.tensor_scalar(out=gm[:, ts, 0:NB], in0=gm[:, ts, 0:NB], scalar1=mx8[:, ts, 2:3], scalar2=None, op0=ALU.is_ge)),
                 reads=['B_mx8', gmn], writes=[gmn])
            P.op('dve', (lambda e: e.tensor_tensor(out=gm[:, ts, 0:NB], in0=gm[:, ts, 0:NB], in1=rw[:, 64 + w0:64 + w0 + NB], op=ALU.mult)),
                 reads=[gmn, 'B_rw'], writes=[gmn])
            P.op('dve', (lambda e: e.tensor_tensor(out=gm[:, ts, 0:NB], in0=gm[:, ts, 0:NB], in1=rw[:, 128 + w0:128 + w0 + NB], op=ALU.add)),
                 reads=[gmn, 'B_rw'], writes=[gmn])
            P.op('dve', (lambda e: e.tensor_scalar(out=mbb[:, ts, 0:NB], in0=gm[:, ts, 0:NB], scalar1=-1.0, scalar2=-NEGM, op0=ALU.add, op1=ALU.mult)),
                 reads=[gmn], writes=[mbn])

        pgate = (ps[7][:, 0:128], 'ps7g')
        ptT = ps[7][:, :].bitcast(BF16)[:, 512:1024]

        def moba_smm(hh, qi, kt, kT, kTn, qT, qTn, rx, rxn, sb_):
            t0 = qi * 512
            diag = kt >= 4 * qi
            n = kt // 2
            dd = 4 * qi - kt + 3
            j = kt - 4 * qi

            def smm(e):
                e.matmul(sb_[0][:, :], kT[:, kt * 128:(kt + 1) * 128], qT[:, t0:t0 + 512], start=True, stop=False)
                if diag:
                    e.matmul(sb_[0][:, :], IDENT, caus[:, j * 512:(j + 1) * 512], start=False, stop=False)
                return e.matmul(sb_[0][:, :], esel[0:35, n * 128:(n + 1) * 128], rx[0:35, :], start=False, stop=True)
            P.op('pe', smm, reads=[kTn, qTn, rxn, 'B_esel', 'B_caus', CONSTS[4]], writes=[sb_[1]])
            pt, ptn = wk['p'].next()
            P.op('act', (lambda e: e.activation(out=pt[:, :], in_=sb_[0][:, :], func=AF.Exp, scale=SCALE,
                                                bias=bcol[:, hh * 64 + dd:hh * 64 + dd + 1])),
                 reads=[sb_[1], 'B_bcol'], writes=[ptn])
            return pt, ptn

        def moba_pv(kt, nkt, v, vn, pt, ptn, pN, pD):
            def pv(e):
                e.matmul(pN[0][:, :], v[:, kt, :], pt[:, :], start=(kt == 0), stop=(kt == nkt - 1))
                return e.matmul(pD[0][:, :], ONES, pt[:, :], start=(kt == 0), stop=(kt == nkt - 1))
            P.op('pe', pv, reads=[vn, ptn, CONSTS[4]], writes=[pN[1], pD[1]])

        def moba_prep1(hh, qi, qT, qTn, km, kmn):
            t0 = qi * 512
            gm, gmn = gmr.next()
            mbb, mbn = mbr.next()
            pg = pgate

            def gmm(e):
                ins = None
                for ts in range(4):
                    ins = e.matmul(pg[0][:, ts * 32:ts * 32 + NB], qT[:, t0 + ts * 128:t0 + (ts + 1) * 128], km[:, :], start=True, stop=True)
                return ins
            P.op('pe', gmm, reads=[qTn, kmn], writes=[pg[1]])
            if NB < 8:
                P.op('dve', (lambda e: e.memset(gm[:, :, :], -3.0e9)), writes=[gmn])
            for ts in range(4):
                moba_sel(qi, ts, gm, gmn, pg, mbb, mbn)
            return mbb, mbn

        def moba_prep2(hh, mbb, mbn):
            rx, rxn = rhx.next()

            def tr(e):
                ins = None
                for ts in range(4):
                    ins = e.transpose(ptT[0:NB, ts * 128:(ts + 1) * 128], mbb[:, ts, 0:NB], IDENT)
                return ins
            P.op('pe', tr, reads=[mbn, CONSTS[4]], writes=['ps7t'])
            P.op('act', (lambda e: e.copy(out=rx[0:NB, :], in_=ptT[0:NB, 0:512])), reads=['ps7t'], writes=[rxn])
            P.op('pool', (lambda e: e.tensor_copy(out=rx[32:35, :], in_=qrow[32:35, hh * 512:(hh + 1) * 512])), reads=['B_qrow'], writes=[rxn])
            return rx, rxn

        def moba_units(hh, qi, kT, kTn, qT, qTn, v, vn, rx, rxn, pN, pD, between=None):
            t0 = qi * 512
            nkt = 4 * qi + 4
            LA = 2
            pts = {}
            for s in range(nkt + LA):
                if s < nkt:
                    pts[s] = moba_smm(hh, qi, s, kT, kTn, qT, qTn, rx, rxn, psS.next())
                if s == 0 and between is not None:
                    between()
                if s - LA >= 0:
                    pt, ptn = pts.pop(s - LA)
                    moba_pv(s - LA, nkt, v, vn, pt, ptn, pN, pD)
            ys, ysn = ystg.next()
            rc, rcn = rcr.next()
            P.op('dve', (lambda e: e.reciprocal(out=rc[:, :], in_=pD[0][:, :])), reads=[pD[1]], writes=[rcn])
            P.op('dve', (lambda e: e.tensor_tensor(out=ys[:, :], in0=pN[0][:, :], in1=rc[:, :], op=ALU.mult)), reads=[pN[1], rcn], writes=[ysn])
            dst = LY[0, hh * 128:(hh + 1) * 128, t0 // TC, (t0 % TC):(t0 % TC) + 512]
            P.op('sp', (lambda e: e.dma_start(out=dst, in_=ys[:, :])), reads=[ysn], writes=['Y'], dma=ysn)

        def moba_head(hh):
            kT, kTn, qT, qTn, v, vn = load_head(hh, 1, 0, 0)
            km, kmn = kmr.next()
            P.op('dve', (lambda e: e.tensor_reduce(out=kmf[:, :], in_=kT[:, :].rearrange("p (n k) -> p n k", k=256), axis=AX.X, op=ALU.add)),
                 reads=[kTn], writes=['B_kmf'])
            P.op('dve', (lambda e: e.tensor_scalar(out=km[:, :], in0=kmf[:, :], scalar1=1.0 / 256, scalar2=None, op0=ALU.mult)),
                 reads=['B_kmf'], writes=[kmn])
            nxt = moba_prep1(hh, 0, qT, qTn, km, kmn)
            for qi in range(NQT):
                rx, rxn = moba_prep2(hh, *nxt)
                holder = {}

                def between(qi=qi):
                    if qi + 1 < NQT:
                        holder['n'] = moba_prep1(hh, qi + 1, qT, qTn, km, kmn)
                pN, pD = psND.next()
                moba_units(hh, qi, kT, kTn, qT, qTn, v, vn, rx, rxn, pN, pD, between)
                nxt = holder.get('n')

        def sb_z(qi, kt, kT, kTn, qT, qTn, zb):
            t0 = qi * 512
            diag = kt >= 4 * qi
            j = kt - 4 * qi

            def zmm(e):
                ins = e.matmul(zb[0][:, :], kT[:, kt * 128:(kt + 1) * 128], qT[:, t0:t0 + 512], start=True, stop=not diag)
                if diag:
                    ins = e.matmul(zb[0][:, :], IDENT, caus[:, (4 + j) * 512:(5 + j) * 512], start=False, stop=True)
                return ins
            P.op('pe', zmm, reads=[kTn, qTn, 'B_caus', CONSTS[4]], writes=[zb[1]])

        def sb_e(zb):
            et, etn = wk['e'].next()
            spt, sptn = wk['sp'].next()
            P.op('act', (lambda e: e.activation(out=et[:, :], in_=zb[0][:, :], func=AF.Exp)), reads=[zb[1]], writes=[etn])
            P.op('act', (lambda e: e.activation(out=spt[:, :], in_=et[:, :], func=AF.Ln, bias=onesb[:, 0:1])), reads=[etn, 'onesb'], writes=[sptn])
            return et, etn, spt, sptn

        def sb_tri(i, spt, sptn, pR):
            P.op('pe', (lambda e: e.matmul(pR[0][:, :], TRI, spt[:, :], start=(i == 0), stop=False, skip_group_check=True)),
                 reads=[sptn, CONSTS[4]], writes=[pR[1]])
            e2, e2n = wk['e2'].next()
            P.op('act', (lambda e: e.activation(out=e2[:, :], in_=pR[0][:, :], func=AF.Exp, scale=-1.0)), reads=[pR[1]], writes=[e2n])
            return e2, e2n

        def sb_ntri(i, nkt, spt, sptn, e2n, pR):
            P.op('pe', (lambda e: e.matmul(pR[0][:, :], NTRI, spt[:, :], start=False, stop=(i == nkt - 1), skip_group_check=True)),
                 reads=[sptn, e2n, CONSTS[4]], writes=[pR[1]])

        def sb_pv(i, nkt, kt, et, etn, e2, e2n, v, vn, pO):
            at, atn = wk['a'].next()
            P.op('dve', (lambda e: e.tensor_tensor(out=at[:, :], in0=et[:, :], in1=e2[:, :], op=ALU.mult)), reads=[etn, e2n], writes=[atn])
            P.op('pe', (lambda e: e.matmul(pO[0][:, :], v[:, kt, :], at[:, :], start=(i == 0), stop=(i == nkt - 1))),
                 reads=[vn, atn], writes=[pO[1]])

        def sb_qtile(hh, qi, kT, kTn, qT, qTn, v, vn, pR, pO):
            t0 = qi * 512
            nkt = 4 * qi + 4
            kts = list(range(nkt - 1, -1, -1))
            zbs, es, e2s = {}, {}, {}
            for s in range(nkt + 5):
                i = s - 3
                if 0 <= i - 1 < nkt:
                    et, etn, spt, sptn = es[i - 1]
                    sb_ntri(i - 1, nkt, spt, sptn, e2s[i - 1][1], pR)
                if 0 <= i < nkt:
                    et, etn, spt, sptn = es[i]
                    e2s[i] = sb_tri(i, spt, sptn, pR)
                if 0 <= i - 1 < nkt:
                    et, etn, spt, sptn = es.pop(i - 1)
                    e2, e2n = e2s.pop(i - 1)
                    sb_pv(i - 1, nkt, kts[i - 1], et, etn, e2, e2n, v, vn, pO)
                if s < nkt:
                    zbs[s] = psZ.next()
                    sb_z(qi, kts[s], kT, kTn, qT, qTn, zbs[s])
                j = s - 1
                if 0 <= j < nkt:
                    es[j] = sb_e(zbs.pop(j))
            ys, ysn = ystg.next()
            P.op('act', (lambda e: e.copy(out=ys[:, :], in_=pO[0][:, :])), reads=[pO[1]], writes=[ysn])
            dst = LY[1, hh * 128:(hh + 1) * 128, t0 // TC, (t0 % TC):(t0 % TC) + 512]
            P.op('sp', (lambda e: e.dma_start(out=dst, in_=ys[:, :])), reads=[ysn], writes=['Y'], dma=ysn)

        def sb_head(hh):
            kT, kTn, qT, qTn, v, vn = load_head(hh, 3, 2, 1)
            for qi in range(NQT):
                sb_qtile(hh, qi, kT, kTn, qT, qTn, v, vn, psR.next(), psO.next())

        psS = Ring([(ps[i], f"ps{i}") for i in range(3)])
        psND = Ring([((ps[3], 'ps3'), (ps[4], 'ps4')), ((ps[5], 'ps5'), (ps[6], 'ps6'))])
        rcr = Ring([(sb(f"B_rcp{i}", [128, 512], F32, st), f"B_rcp{i}") for i in range(2)])
        psZ = Ring([(ps[i], f"ps{i}") for i in range(4)])
        psR = Ring([(ps[4], 'ps4'), (ps[5], 'ps5')])
        psO = Ring([(ps[6], 'ps6'), (ps[7], 'ps7')])
        if 'moba' in PARTS:
            for hh in range(NHC):
                moba_head(hh)
            P.flush()
        if 'sb' in PARTS:
            for hh in range(NHC):
                sb_head(hh)
        P.op('act', (lambda e: e.dma_start(out=SY[:, rk_act, :, :, :].rearrange("b o f th t -> b (o f) th t"), in_=LY)), reads=['Y'], writes=['SY'], dma='xy')
        P.flush()
        st.close()

    def phase_C(l, xsrc, xdst):
        st = ExitStack()
        xt = sb("C_xt", [128, KC, 512], F32, st)
        A1 = sb("C_a1", [128, max(KC, 2 * KCW), 512], BF16, st)
        A2 = sb("C_a2", [128, max(KC, FCH // 128), 512], BF16, st)
        KW = max(KC, KCW, FCH // 128)
        wr = Ring([(sb(f"C_w{i}", [128, KW, 256], BF16, st), f"C_w{i}") for i in range(2)])
        sqr = Ring([(sb(f"C_sq{i}", [128, 4, 512], F32, st), f"C_sq{i}") for i in range(2)])
        rtr = Ring([(sb(f"C_rt{i}", [128, 512], F32, st), f"C_rt{i}") for i in range(2)])
        gr = Ring([(sb(f"C_g{i}", [128, 512], BF16, st), f"C_g{i}") for i in range(3)])
        tmr = Ring([(sb(f"C_t{i}", [128, 512], F32, st), f"C_t{i}") for i in range(3)])
        psg = Ring([(ps[i], f"ps{i}") for i in range(6)])
        psn = Ring([(ps[i], f"ps{i}") for i in (6, 7)])
        ur = Ring([(A2, 'C_a2')])
        P.op('act', (lambda e: e.dma_start(out=CY, in_=SY[:, :, :, rk_act, :].rearrange("b hr f o t -> b hr f (o t)"))), reads=['SY'], writes=['CY'], dma='xy')
        for tp in range(NT):
            t0 = tp * 512
            src = xsrc[:, t0:t0 + 512].rearrange("(kc p) t -> p kc t", p=128)
            P.op('sp', (lambda e, src=src: e.dma_start(out=xt[:, :, :], in_=src)), reads=['X'], writes=['C_xt'], dma='C_xt')
            ysa = CY[0, :, :, t0:t0 + 512].rearrange("hr (kc p) t -> p (hr kc) t", p=128)
            ysb = CY[1, :, :, t0:t0 + 512].rearrange("hr (kc p) t -> p (hr kc) t", p=128)
            P.op('sp', (lambda e, ysa=ysa: e.dma_start(out=A1[:, 0:KCW, :], in_=ysa)), reads=['CY'], writes=['C_a1'], dma='C_a1a')
            P.op('sp', (lambda e, ysb=ysb: e.dma_start(out=A1[:, KCW:2 * KCW, :], in_=ysb)), reads=['CY'], writes=['C_a1'], dma='C_a1b')

            def epi1(mt, pb, t0=t0):
                g, gn = gr.next()
                gsrc = G[mt * 128:(mt + 1) * 128, t0:t0 + 512]
                P.op('sp', (lambda e: e.dma_start(out=g[:, :], in_=gsrc)), reads=['G'], writes=[gn], dma=gn)
                P.op('dve', (lambda e: e.tensor_tensor(out=A2[:, mt, :], in0=pb[0][:, :], in1=g[:, :], op=ALU.mult)), reads=[pb[1], gn], writes=['C_a2'])
            gemm(A1[:, 0:KCW, :], 'C_a1', KCW, wb[("wbm", l)], f"wb_wbm{l}", 0, 0, D, 256, wr, psg, epi1)

            def epi2(mt, pb, t0=t0):
                g, gn = gr.next()
                gsrc = G[D + mt * 128:D + (mt + 1) * 128, t0:t0 + 512]
                P.op('sp', (lambda e: e.dma_start(out=g[:, :], in_=gsrc)), reads=['G'], writes=[gn], dma=gn)
                tm, tmn = tmr.next()
                P.op('dve', (lambda e: e.tensor_tensor(out=tm[:, :], in0=pb[0][:, :], in1=g[:, :], op=ALU.mult)), reads=[pb[1], gn], writes=[tmn])
                P.op('pool', (lambda e: e.tensor_tensor(out=A2[:, mt, :], in0=A2[:, mt, :], in1=tm[:, :], op=ALU.add)), reads=[tmn, 'C_a2'], writes=['C_a2'])
            gemm(A1[:, KCW:2 * KCW, :], 'C_a1', KCW, wb[("wbs", l)], f"wb_wbs{l}", 0, 0, D, 256, wr, psg, epi2)

            def epi3(mt, pb):
                P.op('dve', (lambda e: e.tensor_tensor(out=xt[:, mt, :], in0=xt[:, mt, :], in1=pb[0][:, :], op=ALU.add)), reads=[pb[1], 'C_xt'], writes=['C_xt'])
            gemm(A2[:, 0:KC, :], 'C_a2', KC, wb[("wout", l)], f"wb_wout{l}", 0, 0, D, 256, wr, psg, epi3)
            rmsnorm(xt, A1, gmlp, l * KC, sqr, rtr, psn, 'C_xt', 'C_a1')
            for fc in range(NFC):
                U, Un = ur.next()

                def epi4(mt, pb, U=U, Un=Un):
                    tm, tmn = tmr.next()
                    P.op('act', (lambda e: e.activation(out=tm[:, :], in_=pb[0][:, :], func=AF.Relu)), reads=[pb[1]], writes=[tmn])
                    P.op('pool', (lambda e: e.tensor_tensor(out=U[:, mt, :], in0=tm[:, :], in1=tm[:, :], op=ALU.mult)), reads=[tmn], writes=[Un])
                gemm(A1[:, 0:KC, :], 'C_a1', KC, wb[("wup", l)], f"wb_wup{l}", 0, fc * FCH, FCH, 256, wr, psg, epi4)
                gemm(U[:, 0:FCH // 128, :], Un, FCH // 128, wb[("wdn", l)], f"wb_wdn{l}", fc * FCH, 0, D, 256, wr, psg, epi3)
            dst = xdst[:, t0:t0 + 512].rearrange("(kc p) t -> p kc t", p=128)
            P.op('sp', (lambda e, dst=dst: e.dma_start(out=dst, in_=xt[:, :, :])), reads=['C_xt'], writes=['X'], dma='C_xst')
        P.flush()
        st.close()

    dbg_out = {}
    for l in range(NL):
        xsrc = xT if l == 0 else X1
        xdst = X1 if l < NL - 1 else outT
        if 'A' in PARTS:
            phase_A(l, xsrc)
        P.barrier()
        phase_B(l)
        P.barrier()
        if 'C' in PARTS:
            phase_C(l, xsrc, xdst)
        if dbg and l == dbg - 1:
            for nm, t in (("LQK", LQK), ("LV", LV), ("LY", LY), ("G", G)):
                o = nc.dram_tensor("dbg_" + nm, list(t.shape), BF16, kind="ExternalOutput").ap()
                P.op('sp', (lambda e, o=o, t=t: e.dma_start(out=o, in_=t)), reads=['QKV', 'Y', 'G'], writes=['dbg' + nm], dma='dbg' + nm)
            break
    P.flush()
    return nc


def _tables(cfg, r):
    import ml_dtypes
    bf = ml_dtypes.bfloat16
    NB, NHC, NH = cfg.NB, cfg.NHC, cfg.NH
    scale = 128.0 ** -0.5
    slopes_all = 2.0 ** (-8.0 * np.arange(1, NH + 1, dtype=np.float64) / NH)
    sl = slopes_all[r * NHC:(r + 1) * NHC]
    k = np.arange(128, dtype=np.float64)[:, None]
    dd = np.arange(64, dtype=np.float64)[None, :] - 3.0
    bcol = np.concatenate([(s * (k - 128.0 * dd)) for s in sl], axis=1).astype(np.float32)
    w = np.arange(64)
    rowneg = np.where(w < 32, 0.0, -1.0e9)
    rowpast = np.where(w < 32, 1.0, 0.0)
    rowown = np.where(w == 32, 1.0, 0.0)
    rw = np.tile(np.concatenate([rowneg, rowpast, rowown])[None, :], (128, 1)).astype(np.float32)
    esel = np.zeros((35, NB, 128), np.float32)
    for n in range(min(NB, 32)):
        esel[n, n, :] = 1.0
    esel[32:35, :, :] = 1.0
    esel = esel.reshape(35, NB * 128).astype(bf)
    qrow = np.zeros((35, NHC, 512), np.float32)
    i = np.arange(512, dtype=np.float64)
    for hh, s in enumerate(sl):
        val = -s * i / scale
        a = val.astype(np.float32).astype(bf)
        rem = val - a.astype(np.float64)
        b = rem.astype(np.float32).astype(bf)
        rem2 = rem - b.astype(np.float64)
        c = rem2.astype(np.float32).astype(bf)
        qrow[32, hh], qrow[33, hh], qrow[34, hh] = a.astype(np.float32), b.astype(np.float32), c.astype(np.float32)
    qrow = qrow.reshape(35, NHC * 512).astype(bf)
    caus = np.zeros((128, 2, 4, 512), np.float32)
    kk = np.arange(128)[:, None]
    tt = np.arange(512)[None, :]
    for j in range(4):
        kp = j * 128 + kk
        caus[:, 0, j, :] = np.where(kp <= tt, 0.0, NEGM)
        caus[:, 1, j, :] = np.where(kp < tt, 0.0, NEGM)
    caus = caus.reshape(128, 2 * 4 * 512).astype(bf)
    ident = np.eye(128, dtype=np.float32)
    tri = (np.arange(128)[:, None] >= np.arange(128)[None, :]).astype(np.float32)
    ntri = 1.0 - tri
    ones = np.ones((128, 128), np.float32)
    cst = np.concatenate([ident, tri, ntri, ones], axis=1).astype(bf)
    return dict(bcol=bcol, rw=rw, esel=esel, qrow=qrow, caus=caus, cst=cst, onesf=np.ones((128, 128), np.float32))


def _pvec(v, KCn):
    L = v.shape[0]
    return np.ascontiguousarray(v.reshape(L, KCn, 128).transpose(2, 0, 1).reshape(128, L * KCn)).astype(np.float32)


def make_in_maps(cfg, x, norm_mix, w_in, b_gate, q_norm, k_norm, w_branch_moba, w_branch_sb, w_out, norm_mlp, w_up, w_down):
    NL, D, TC = cfg.NL, cfg.D, cfg.TC
    ws = {"win": w_in, "wbm": w_branch_moba, "wbs": w_branch_sb, "wout": w_out, "wup": w_up, "wdn": w_down}
    tabs = [_tables(cfg, r) for r in range(2)]
    gq = np.stack([np.asarray(q_norm), np.asarray(k_norm)], axis=1)
    gqk = np.ascontiguousarray(gq.transpose(2, 0, 1).reshape(128, NL * 2)).astype(np.float32)
    common = dict(gmix=_pvec(np.asarray(norm_mix), cfg.KC), gmlp=_pvec(np.asarray(norm_mlp), cfg.KC),
                  bgate=_pvec(np.asarray(b_gate), 2 * cfg.KC), gqk=gqk)
    halves = {}
    for nm, w in ws.items():
        w = np.asarray(w)
        Kh = w.shape[1] // 2
        for l in range(NL):
            for r in range(2):
                halves[(nm, l, r)] = np.ascontiguousarray(w[l, r * Kh:(r + 1) * Kh, :])
    xTs = {}
    for b in range(2):
        for r in range(2):
            xTs[(b, r)] = np.ascontiguousarray(np.asarray(x)[b, r * TC:(r + 1) * TC, :].T)
    epoch = int(np.random.randint(1, 1 << 24))
    common["barv"] = (epoch * 16 + np.arange(16)).astype(np.int32)[None, :]
    in_maps = []
    zeros = {}
    for c in range(8):
        b, r = c // 4, c % 2
        live = (c % 4) < 2
        m = dict(common)
        m.update(tabs[r])
        real = {"xT": xTs[(b, r)]}
        for nm in ws:
            for l in range(NL):
                real[f"{nm}{l}"] = halves[(nm, l, r)]
        for k_, v_ in real.items():
            if live:
                m[k_] = v_
            else:
                if v_.shape not in zeros:
                    zeros[v_.shape] = np.zeros(v_.shape, np.float32)
                m[k_] = zeros[v_.shape]
        in_maps.append(m)
    return in_maps


_CACHE = {}


def kernel(x, norm_mix, w_in, b_gate, q_norm, k_norm, w_branch_moba, w_branch_sb, w_out, norm_mlp, w_up, w_down):
    cfg = Cfg()
    if 'nc' not in _CACHE:
        _CACHE['nc'] = build(cfg)
    nc = _CACHE['nc']
    in_maps = make_in_maps(cfg, x, norm_mix, w_in, b_gate, q_norm, k_norm, w_branch_moba, w_branch_sb, w_out, norm_mlp, w_up, w_down)
    res = run_bass_kernel_spmd(nc, in_maps, core_ids=list(range(8)))
    out = np.empty((2, cfg.S, cfg.D), np.float32)
    for c in (0, 1, 4, 5):
        b, r = c // 4, c % 2
        out[b, r * cfg.TC:(r + 1) * cfg.TC, :] = res.results[c]["outT"].T
    return out
```
